# Optimizing a Trainium2 kernel written in Bass

```python
import math
import jax, jax.numpy as jnp
from jax import lax
import numpy as np

D_MODEL = 1024
BATCH = 8
SEQ = 2048
DEPTH = 2

CHUNK = 64
RET_HEADS = 8
RET_QK = D_MODEL // 2
RET_V = D_MODEL
RET_QK_DIM = RET_QK // RET_HEADS
RET_V_DIM = RET_V // RET_HEADS
ROPE_BASE = 10000.0
S5_WIDTH = D_MODEL // 2
S5_GROUP_CH = 16
S5_GROUPS = S5_WIDTH // S5_GROUP_CH
S5_STATE = 64
MOE_GROUPS = 4
EXPERTS_PER_GROUP = 8
N_EXPERTS = MOE_GROUPS * EXPERTS_PER_GROUP
TOP_K_IN_GROUP = 2
EXPERT_FF = D_MODEL // 4
LN_EPS = 1e-5
HEAD_NORM_EPS = 1e-6
DN_ALPHA = (2 * DEPTH) ** 0.25
DN_BETA = (8 * DEPTH) ** -0.25
IN_WIDTH = 2 * RET_QK + 2 * RET_V + S5_WIDTH + 2 * D_MODEL
IN_SPLITS = (RET_QK, 2 * RET_QK, 2 * RET_QK + RET_V, 2 * RET_QK + 2 * RET_V,
             2 * RET_QK + 2 * RET_V + S5_WIDTH, 2 * RET_QK + 2 * RET_V + S5_WIDTH + D_MODEL)

kernel_name = 'hybrid_retention_s5_hmoe'


def layer_norm(x, g, b):
    xf = x.astype(jnp.float32)
    mu = jnp.mean(xf, -1, keepdims=True)
    var = jnp.mean(jnp.square(xf - mu), -1, keepdims=True)
    return ((xf - mu) * lax.rsqrt(var + LN_EPS) * g.astype(jnp.float32) + b.astype(jnp.float32)).astype(x.dtype)


def rope_tables(seq_len, dim):
    half = dim // 2
    inv_freq = ROPE_BASE ** (-jnp.arange(half, dtype=jnp.float32) / half)
    ang = jnp.arange(seq_len, dtype=jnp.float32)[:, None] * inv_freq[None, :]
    return jnp.cos(ang), jnp.sin(ang)


def rotary(x, cos, sin):
    x1, x2 = jnp.split(x, 2, axis=-1)
    c = cos[None, :, None, :]
    s = sin[None, :, None, :]
    return jnp.concatenate([x1 * c - x2 * s, x1 * s + x2 * c], axis=-1)


def chunk_retention(q, k, v):
    b, s, h, dk = q.shape
    dv = v.shape[-1]
    nc = s // CHUNK
    log_gamma = jnp.log1p(-(2.0 ** (-5.0 - jnp.arange(h, dtype=jnp.float32))))
    pos = jnp.arange(CHUNK, dtype=jnp.float32)
    intra = jnp.exp(log_gamma[:, None, None] * jnp.abs(pos[:, None] - pos[None, :]))
    k_decay = jnp.exp(log_gamma[None, :] * (CHUNK - 1.0 - pos)[:, None])
    q_decay = jnp.exp(log_gamma[None, :] * (pos + 1.0)[:, None])
    chunk_decay = jnp.exp(log_gamma * CHUNK)
    qc = q.reshape(b, nc, CHUNK, h, dk)
    kc = k.reshape(b, nc, CHUNK, h, dk) * (dk ** -0.5)
    vc = v.reshape(b, nc, CHUNK, h, dv)
    scores = jnp.einsum('bnihd,bnjhd->bnhij', qc, kc) * intra[None, None]
    o_intra = jnp.einsum('bnhij,bnjhe->bnihe', scores, vc)
    kv = jnp.einsum('bnjhd,bnjhe->nbhde', kc * k_decay[:, :, None], vc)

    def step(state, kv_n):
        return chunk_decay[None, :, None, None] * state + kv_n, state

    _, states = lax.scan(step, jnp.zeros_like(kv[0]), kv)
    o_inter = jnp.einsum('bnihd,nbhde->bnihe', qc * q_decay[:, :, None], states)
    return (o_intra + o_inter).reshape(b, s, h, dv)


def s5_ssm(u, lam_re, lam_im, log_step, b_re, b_im, c_re, c_im, d_skip):
    f32 = jnp.float32
    bsz, s, _ = u.shape
    lam_re = jnp.minimum(lam_re.astype(f32), -1e-4)
    lam_im = lam_im.astype(f32)
    b_re, b_im = b_re.astype(f32), b_im.astype(f32)
    c_re, c_im = c_re.astype(f32), c_im.astype(f32)
    ug = u.reshape(bsz, s, S5_GROUPS, S5_GROUP_CH)
    step = jnp.exp(log_step.astype(f32))[:, None]
    mag = jnp.exp(lam_re * step)
    ang = lam_im * step
    ab_re = mag * jnp.cos(ang)
    ab_im = mag * jnp.sin(ang)
    den = lam_re * lam_re + lam_im * lam_im
    n_re = ab_re - 1.0
    zc_re = (n_re * lam_re + ab_im * lam_im) / den
    zc_im = (ab_im * lam_re - n_re * lam_im) / den
    bb_re = zc_re[..., None] * b_re - zc_im[..., None] * b_im
    bb_im = zc_re[..., None] * b_im + zc_im[..., None] * b_re
    bu_re = jnp.einsum('bsgc,gnc->bsgn', ug, bb_re)
    bu_im = jnp.einsum('bsgc,gnc->bsgn', ug, bb_im)
    a_re = jnp.broadcast_to(ab_re[None, None], (1, s, S5_GROUPS, S5_STATE))
    a_im = jnp.broadcast_to(ab_im[None, None], (1, s, S5_GROUPS, S5_STATE))

    def combine(e1, e2):
        a1r, a1i, b1r, b1i = e1
        a2r, a2i, b2r, b2i = e2
        return (a2r * a1r - a2i * a1i, a2r * a1i + a2i * a1r,
                a2r * b1r - a2i * b1i + b2r, a2r * b1i + a2i * b1r + b2i)

    _, _, h_re, h_im = lax.associative_scan(combine, (a_re, a_im, bu_re, bu_im), axis=1)
    y = jnp.einsum('bsgn,gcn->bsgc', h_re, c_re) - jnp.einsum('bsgn,gcn->bsgc', h_im, c_im)
    return y.reshape(bsz, s, S5_WIDTH) + d_skip.astype(f32) * u


def hybrid_mixer(h, w_in, lam_re, lam_im, log_step, b_re, b_im, c_re, c_im, d_skip,
                 w_glu, w_branch_ret, w_branch_s5, w_out, cos, sin):
    f32 = jnp.float32
    bsz, s, _ = h.shape
    proj = h @ w_in
    q, k, v, g_ret, u, gate_ret, gate_s5 = jnp.split(proj, IN_SPLITS, axis=-1)
    q = rotary(q.reshape(bsz, s, RET_HEADS, RET_QK_DIM).astype(f32), cos, sin)
    k = rotary(k.reshape(bsz, s, RET_HEADS, RET_QK_DIM).astype(f32), cos, sin)
    v = v.reshape(bsz, s, RET_HEADS, RET_V_DIM).astype(f32)
    o = chunk_retention(q, k, v)
    mu = jnp.mean(o, -1, keepdims=True)
    var = jnp.mean(jnp.square(o - mu), -1, keepdims=True)
    o = ((o - mu) * lax.rsqrt(var + HEAD_NORM_EPS)).reshape(bsz, s, RET_V).astype(h.dtype)
    ret_branch = (jax.nn.silu(g_ret) * o) @ w_branch_ret
    y = s5_ssm(u.astype(f32), lam_re, lam_im, log_step, b_re, b_im, c_re, c_im, d_skip).astype(h.dtype)
    z = jax.nn.gelu(y)
    s5_branch = (z * jax.nn.sigmoid(z @ w_glu)) @ w_branch_s5
    merged = jax.nn.sigmoid(gate_ret) * ret_branch + jax.nn.sigmoid(gate_s5) * s5_branch
    return merged @ w_out


def hier_moe(h, w_rg, b_rg, w_re, b_re, w_gate, w_up, w_down):
    f32 = jnp.float32
    bsz, s, d = h.shape
    xt = h.reshape(-1, d)
    t = xt.shape[0]
    g_prob = jax.nn.softmax((xt @ w_rg + b_rg).astype(f32), axis=-1)
    g_top, g_idx = lax.top_k(g_prob, 1)
    g_onehot = jax.nn.one_hot(g_idx[:, 0], MOE_GROUPS, dtype=f32)
    e_logits = (xt @ w_re + b_re).astype(f32).reshape(t, MOE_GROUPS, EXPERTS_PER_GROUP)
    e_logits = jnp.einsum('tge,tg->te', e_logits, g_onehot)
    e_prob = jax.nn.softmax(e_logits, axis=-1)
    e_top, e_idx = lax.top_k(e_prob, TOP_K_IN_GROUP)
    e_top = e_top / jnp.sum(e_top, -1, keepdims=True)
    w_local = jnp.sum(jax.nn.one_hot(e_idx, EXPERTS_PER_GROUP, dtype=f32) * e_top[..., None], axis=1)
    comb = (g_onehot[:, :, None] * (g_top[:, :, None] * w_local[:, None, :])).astype(h.dtype)
    out = jnp.zeros_like(xt)
    for gi in range(MOE_GROUPS):
        sl = slice(gi * EXPERTS_PER_GROUP, (gi + 1) * EXPERTS_PER_GROUP)
        act = jax.nn.silu(jnp.einsum('td,edf->tef', xt, w_gate[sl])) * jnp.einsum('td,edf->tef', xt, w_up[sl])
        out = out + jnp.einsum('tef,efd->td', act * comb[:, gi, :, None], w_down[sl])
    return out.reshape(bsz, s, d)


def setup_inputs(seed: int = 0) -> dict:
    key = jax.random.key(seed)
    ks = jax.random.split(key, 32)
    f32 = jnp.float32
    L, D = DEPTH, D_MODEL
    G, N, GC = S5_GROUPS, S5_STATE, S5_GROUP_CH

    def nrm(k, shape, scale):
        return scale * jax.random.normal(k, shape, f32)

    col_scale = jnp.concatenate([jnp.ones((2 * RET_QK,), f32), jnp.full((RET_V,), DN_BETA, f32),
                                 jnp.ones((IN_WIDTH - 2 * RET_QK - RET_V,), f32)])
    return {
        'x': jax.random.normal(ks[0], (BATCH, SEQ, D), f32),
        'ln_in_g': 1.0 + nrm(ks[1], (D,), 0.01),
        'ln_in_b': nrm(ks[2], (D,), 0.01),
        'w_in': nrm(ks[3], (L, D, IN_WIDTH), D ** -0.5) * col_scale,
        's5_lambda_re': -0.5 + nrm(ks[4], (L, G, N), 0.01),
        's5_lambda_im': math.pi * jnp.arange(N, dtype=f32) + nrm(ks[5], (L, G, N), 0.01),
        's5_log_step': jax.random.uniform(ks[6], (L, G), f32, math.log(1e-3), math.log(1e-1)),
        's5_b_re': nrm(ks[7], (L, G, N, GC), (2.0 * GC) ** -0.5),
        's5_b_im': nrm(ks[8], (L, G, N, GC), (2.0 * GC) ** -0.5),
        's5_c_re': nrm(ks[9], (L, G, GC, N), N ** -0.5),
        's5_c_im': nrm(ks[10], (L, G, GC, N), N ** -0.5),
        's5_d': nrm(ks[11], (L, S5_WIDTH), 1.0),
        'w_glu': nrm(ks[12], (L, S5_WIDTH, S5_WIDTH), S5_WIDTH ** -0.5),
        'w_branch_ret': nrm(ks[13], (L, RET_V, D), RET_V ** -0.5 * DN_BETA),
        'w_branch_s5': nrm(ks[14], (L, S5_WIDTH, D), S5_WIDTH ** -0.5 * DN_BETA),
        'w_out': nrm(ks[15], (L, D, D), D ** -0.5 * DN_BETA),
        'ln_mix_g': 1.0 + nrm(ks[16], (L, D), 0.01),
        'ln_mix_b': nrm(ks[17], (L, D), 0.01),
        'w_router_group': nrm(ks[18], (L, D, MOE_GROUPS), D ** -0.5),
        'b_router_group': nrm(ks[19], (L, MOE_GROUPS), 0.01),
        'w_router_expert': nrm(ks[20], (L, D, N_EXPERTS), D ** -0.5),
        'b_router_expert': nrm(ks[21], (L, N_EXPERTS), 0.01),
        'w_exp_gate': nrm(ks[22], (L, N_EXPERTS, D, EXPERT_FF), D ** -0.5),
        'w_exp_up': nrm(ks[23], (L, N_EXPERTS, D, EXPERT_FF), D ** -0.5 * DN_BETA),
        'w_exp_down': nrm(ks[24], (L, N_EXPERTS, EXPERT_FF, D), EXPERT_FF ** -0.5 * DN_BETA),
        'ln_ffn_g': 1.0 + nrm(ks[25], (L, D), 0.01),
        'ln_ffn_b': nrm(ks[26], (L, D), 0.01),
    }


def reference(x, ln_in_g, ln_in_b, w_in, s5_lambda_re, s5_lambda_im, s5_log_step,
              s5_b_re, s5_b_im, s5_c_re, s5_c_im, s5_d, w_glu, w_branch_ret, w_branch_s5,
              w_out, ln_mix_g, ln_mix_b, w_router_group, b_router_group, w_router_expert,
              b_router_expert, w_exp_gate, w_exp_up, w_exp_down, ln_ffn_g, ln_ffn_b):
    cos, sin = rope_tables(x.shape[1], RET_QK_DIM)
    h = layer_norm(x, ln_in_g, ln_in_b)
    for l in range(DEPTH):
        mix = hybrid_mixer(h, w_in[l], s5_lambda_re[l], s5_lambda_im[l], s5_log_step[l],
                           s5_b_re[l], s5_b_im[l], s5_c_re[l], s5_c_im[l], s5_d[l],
                           w_glu[l], w_branch_ret[l], w_branch_s5[l], w_out[l], cos, sin)
        h = layer_norm(DN_ALPHA * h + mix, ln_mix_g[l], ln_mix_b[l])
        ffn = hier_moe(h, w_router_group[l], b_router_group[l], w_router_expert[l],
                       b_router_expert[l], w_exp_gate[l], w_exp_up[l], w_exp_down[l])
        h = layer_norm(DN_ALPHA * h + ffn, ln_ffn_g[l], ln_ffn_b[l])
    return h
```

```python
import math
import os
from contextlib import ExitStack
import numpy as np
import concourse.bass as bass
import concourse.mybir as mybir
from concourse.bass_utils import run_bass_kernel_spmd

F32 = mybir.dt.float32
BF16 = mybir.dt.bfloat16
I32 = mybir.dt.int32
AF = mybir.ActivationFunctionType
ALU = mybir.AluOpType
AX = mybir.AxisListType

D = 1024
SEQ = 2048
DEPTH = 2
NT = 16
INW = 5632
QOFF, KOFF, VOFF, GOFF, UOFF, GROFF, GSOFF = 0, 512, 1024, 2048, 3072, 3584, 4608
NEXP = 32
ALPHA = (2 * DEPTH) ** 0.25
LN_EPS = 1e-5
HN_EPS = 1e-6
TWO_PI = 2.0 * math.pi
POWS = [1, 2, 3, 4, 5, 6, 7, 8, 16, 32, 64, 128, 256, 512, 1024]

DEBUG = {"dumps": None, "stop": None}


class Sched:
    ENGS = ("pe", "act", "dve", "pool", "sp")

    def __init__(self, nc):
        self.nc = nc
        self.ops = []

    def add(self, eng, fn, reads=(), writes=(), dma=None, nobar=False):
        self.ops.append(dict(eng=eng, fn=fn, reads=tuple(reads), writes=tuple(writes), dma=dma, bar=False, nobar=nobar))

    def pe(self, fn, reads=(), writes=()):
        self.add("pe", fn, reads, writes)

    def act(self, fn, reads=(), writes=()):
        self.add("act", fn, reads, writes)

    def dve(self, fn, reads=(), writes=()):
        self.add("dve", fn, reads, writes)

    def pool(self, fn, reads=(), writes=()):
        self.add("pool", fn, reads, writes)

    def dma(self, eng, fn, reads=(), writes=(), key=None, nobar=False):
        if key is None:
            key = writes[0]
        self.add(eng, fn, reads, writes, dma=key, nobar=nobar)

    def barrier(self):
        self.ops.append(dict(eng=None, fn=None, reads=(), writes=(), dma=None, bar=True, nobar=False))

    def emit(self, stack):
        nc = self.nc
        ops = self.ops
        n = len(ops)
        last_w, readers = {}, {}
        deps = [()] * n
        signals = [False] * n
        last_on = {e: None for e in self.ENGS}
        bar_deps = []
        seen_dma = []
        for i, op in enumerate(ops):
            if op["bar"]:
                bar_deps = [j for j in last_on.values() if j is not None] + list(seen_dma)
                seen_dma = []
                continue
            d = set() if op["nobar"] else set(bar_deps)
            raw = set()
            for r in op["reads"]:
                if r in last_w:
                    d.add(last_w[r]); raw.add(last_w[r])
            for w in op["writes"]:
                if w in last_w:
                    d.add(last_w[w])
                d.update(readers.get(w, ()))
            need = []
            for j in d:
                oj = ops[j]
                if oj["dma"] is not None:
                    need.append(j)
                elif oj["eng"] == op["eng"]:
                    if op["dma"] is not None or op["eng"] != "pe":
                        need.append(j)
                else:
                    need.append(j)
            for j in need:
                if ops[j]["dma"] is None:
                    signals[j] = True
            deps[i] = need
            for r in op["reads"]:
                readers.setdefault(r, []).append(i)
            for w in op["writes"]:
                last_w[w] = i
                readers[w] = []
            if not op["nobar"]:
                last_on[op["eng"]] = i
            if op["dma"] is not None:
                seen_dma.append(i)
        esem = {e: stack.enter_context(nc.semaphore(f"s_{e}")) for e in self.ENGS}
        dkeys = []
        for op in ops:
            if op["dma"] is not None and op["dma"] not in dkeys:
                dkeys.append(op["dma"])
        dsem = {k: stack.enter_context(nc.semaphore(f"d_{idx}")) for idx, k in enumerate(dkeys)}
        cnt = {e: 0 for e in self.ENGS}
        dcnt = {k: 0 for k in dkeys}
        sigval = [0] * n
        for i, op in enumerate(ops):
            if op["bar"]:
                continue
            if op["dma"] is not None:
                dcnt[op["dma"]] += 16
                sigval[i] = dcnt[op["dma"]]
            elif signals[i]:
                cnt[op["eng"]] += 1
                sigval[i] = cnt[op["eng"]]
        waited = {e: {} for e in self.ENGS}
        plan = {e: [] for e in self.ENGS}
        dtot = {k: 0 for k in dkeys}
        nwaits = 0
        for i, op in enumerate(ops):
            if op["bar"]:
                continue
            if op["dma"] is not None:
                dtot[op["dma"]] += 16
            e = op["eng"]
            ws = {}
            for j in deps[i]:
                oj = ops[j]
                if oj["dma"] is not None:
                    key = ("d", oj["dma"])
                    val = dtot[oj["dma"]] if op["dma"] != oj["dma"] else sigval[j]
                else:
                    key = ("e", oj["eng"])
                    val = sigval[j]
                if waited[e].get(key, 0) >= val:
                    continue
                ws[key] = max(ws.get(key, 0), val)
            for key, val in ws.items():
                waited[e][key] = val
            nwaits += len(ws)
            plan[e].append((i, list(ws.items())))
        self.stats = dict(n_ops={e: len(plan[e]) for e in self.ENGS}, signals=dict(cnt), dma_keys=len(dkeys), waits=nwaits)
        block = stack.enter_context(nc.Block())

        def run(engname):
            def body(eng):
                for i, ws in plan[engname]:
                    op = ops[i]
                    for key, val in ws:
                        eng.wait_ge(dsem[key[1]] if key[0] == "d" else esem[key[1]], val)
                    inst = op["fn"](eng)
                    if op["dma"] is not None:
                        inst.then_inc(dsem[op["dma"]], 16)
                    elif signals[i]:
                        inst.then_inc(esem[engname], 1)
                for k in dkeys:
                    if any((not o["bar"]) and o["dma"] == k and o["eng"] == engname for o in ops):
                        eng.wait_ge(dsem[k], dcnt[k])
            return body

        block.tensor(run("pe"))
        block.scalar(run("act"))
        block.vector(run("dve"))
        block.gpsimd(run("pool"))
        block.sync(run("sp"))


def gammas():
    return [1.0 - 2.0 ** (-5.0 - h) for h in range(8)]


def host_constants():
    c = {}
    c["c_ident"] = np.eye(128, dtype=np.float32)
    half = 32
    inv_freq = (10000.0 ** (-np.arange(half, dtype=np.float32) / half)).astype(np.float32)
    ang = np.arange(SEQ, dtype=np.float32)[:, None] * inv_freq[None, :]
    cos, sin = np.cos(ang).astype(np.float32), np.sin(ang).astype(np.float32)
    rope = np.zeros((2, 128, SEQ), np.float32)
    for p in range(128):
        d = p % 64
        f = d % 32
        rope[0, p] = cos[:, f]
        rope[1, p] = -sin[:, f] if d < 32 else sin[:, f]
    c["c_rope"] = rope
    g = gammas()
    intra = np.zeros((128, 8, 128), np.float32)
    jj = np.arange(128)
    for h in range(8):
        m = (g[h] ** np.abs(jj[:, None] - jj[None, :])) * 0.125
        m = m * ((jj[:, None] // 64) == (jj[None, :] // 64))
        intra[:, h, :] = m
    c["c_intra"] = intra
    kdec = np.zeros((128, 2, 8), np.float32)
    for h in range(8):
        for j in range(64):
            kdec[j, 0, h] = (g[h] ** (63 - j)) * 0.125
            kdec[64 + j, 1, h] = (g[h] ** (63 - j)) * 0.125
    c["c_kdec"] = kdec
    qdec = np.zeros((128, 4, 64), np.float32)
    for p in range(128):
        for c4 in range(4):
            h = 2 * c4 + p // 64
            qdec[p, c4, :] = g[h] ** (np.arange(64) + 1.0)
    c["c_qdec"] = qdec
    sdec = np.zeros((128, 8), np.float32)
    for h in range(8):
        sdec[:, h] = g[h] ** 64
    c["c_sdec"] = sdec
    c["c_ones"] = np.full((128, 128), 1.0 / 128.0, np.float32)
    return c


def host_layout(inp):
    L = DEPTH
    o = {}
    f = lambda a: np.ascontiguousarray(np.asarray(a, dtype=np.float32))
    w_in = f(inp["w_in"])
    o["w_in"] = w_in
    perm = np.zeros(512, np.int64)
    for h in range(8):
        for d in range(64):
            perm[h * 64 + d] = h * 64 + (d + 32 if d < 32 else d - 32)
    o["w_qksw"] = np.ascontiguousarray(np.concatenate([w_in[:, :, QOFF + perm], w_in[:, :, KOFF + perm]], axis=2))
    for k in ("w_glu", "w_branch_ret", "w_branch_s5", "w_out", "w_exp_gate", "w_exp_up", "w_exp_down"):
        o[k] = f(inp[k])
    o["w_router"] = np.ascontiguousarray(np.concatenate([f(inp["w_router_group"]), f(inp["w_router_expert"])], axis=2))
    o["b_router"] = np.ascontiguousarray(np.concatenate([f(inp["b_router_group"]), f(inp["b_router_expert"])], axis=1))
    lng = np.stack([f(inp["ln_in_g"])] + [f(inp["ln_mix_g"])[l] for l in range(L)] + [f(inp["ln_ffn_g"])[l] for l in range(L)])
    lnb = np.stack([f(inp["ln_in_b"])] + [f(inp["ln_mix_b"])[l] for l in range(L)] + [f(inp["ln_ffn_b"])[l] for l in range(L)])
    o["ln_gb"] = np.ascontiguousarray(np.stack([lng, lnb], axis=1))
    bre, bim = f(inp["s5_b_re"]), f(inp["s5_b_im"])
    cre, cim = f(inp["s5_c_re"]), f(inp["s5_c_im"])
    BB = np.zeros((L, 2, 128, 16, 128), np.float32)
    CC = np.zeros((L, 2, 128, 16, 128), np.float32)
    for g in range(32):
        j, g2, gl = g // 2, g % 2, g % 8
        BB[:, 0, gl * 16:(gl + 1) * 16, j, g2 * 64:(g2 + 1) * 64] = np.transpose(bre[:, g], (0, 2, 1))
        BB[:, 1, gl * 16:(gl + 1) * 16, j, g2 * 64:(g2 + 1) * 64] = np.transpose(bim[:, g], (0, 2, 1))
        CC[:, 0, g2 * 64:(g2 + 1) * 64, j, gl * 16:(gl + 1) * 16] = np.transpose(cre[:, g], (0, 2, 1))
        CC[:, 1, g2 * 64:(g2 + 1) * 64, j, gl * 16:(gl + 1) * 16] = np.transpose(cim[:, g], (0, 2, 1))
    o["s5_BB"], o["s5_CC"] = BB, CC
    lre, lim, lst = f(inp["s5_lambda_re"]), f(inp["s5_lambda_im"]), f(inp["s5_log_step"])
    lste = np.repeat(lst[:, :, None], 64, axis=2)
    o["s5_lamb"] = np.ascontiguousarray(np.stack([lre, lim, lste], axis=1).reshape(L, 3, 2048))
    lamp = np.zeros((L, 128, 3, 16), np.float32)
    for g in range(32):
        j, g2 = g // 2, g % 2
        lamp[:, g2 * 64:(g2 + 1) * 64, 0, j] = lre[:, g]
        lamp[:, g2 * 64:(g2 + 1) * 64, 1, j] = lim[:, g]
        lamp[:, g2 * 64:(g2 + 1) * 64, 2, j] = lste[:, g]
    o["s5_lamp"] = lamp
    o["s5_dcol"] = np.ascontiguousarray(np.transpose(f(inp["s5_d"]).reshape(L, 4, 128), (0, 2, 1)))
    return o


PARAM_SHAPES = {
    "w_in": [DEPTH, D, INW], "w_qksw": [DEPTH, D, 1024], "w_glu": [DEPTH, 512, 512],
    "w_branch_ret": [DEPTH, D, D], "w_branch_s5": [DEPTH, 512, D], "w_out": [DEPTH, D, D],
    "w_exp_gate": [DEPTH, NEXP, D, 256], "w_exp_up": [DEPTH, NEXP, D, 256], "w_exp_down": [DEPTH, NEXP, 256, D],
    "w_router": [DEPTH, D, 36], "b_router": [DEPTH, 36], "ln_gb": [1 + 2 * DEPTH, 2, D],
    "s5_BB": [DEPTH, 2, 128, 16, 128], "s5_CC": [DEPTH, 2, 128, 16, 128], "s5_lamb": [DEPTH, 3, 2048],
    "s5_lamp": [DEPTH, 128, 3, 16], "s5_dcol": [DEPTH, 128, 4],
    "c_ident": [128, 128], "c_rope": [2, 128, SEQ], "c_intra": [128, 8, 128], "c_kdec": [128, 2, 8],
    "c_qdec": [128, 4, 64], "c_sdec": [128, 8], "c_ones": [128, 128],
}


class Builder:
    def __init__(self, nc, st):
        self.nc, self.st = nc, st
        self.S = Sched(nc)
        self.dumps = {}
        nc_ = nc
        self.x = nc.dram_tensor("x", [SEQ, D], F32, kind="ExternalInput").ap()
        self.out = nc.dram_tensor("out", [SEQ, D], F32, kind="ExternalOutput").ap()
        self.dr = {k: nc.dram_tensor(k, shp, F32, kind="ExternalInput").ap() for k, shp in PARAM_SHAPES.items()}
        self.spillA = nc.dram_tensor("spillA", [SEQ, D], F32, kind="Internal").ap()
        self.spillB = nc.dram_tensor("spillB", [SEQ, D], F32, kind="Internal").ap()
        sb = lambda name, shape, dt: st.enter_context(nc.sbuf_tensor(name, shape, dt))
        self.hT = sb("hT", [128, 8, SEQ], BF16)
        self.BIG = sb("BIG", [128, 16384], F32)
        self.uz = sb("uz", [128, 4, SEQ], BF16)
        self.Y16 = sb("Y16", [128, 8192], BF16)
        self.wsl = sb("wsl", [128, 6, 8, 256], BF16)
        self.PH = sb("PH", [128, 4096], F32)
        self.T8 = sb("T8", [128, 4, 512], F32)
        self.ident_f = sb("ident_f", [128, 128], F32)
        self.ident_b = sb("ident_b", [128, 128], BF16)
        self.ones_f = sb("ones_f", [128, 128], F32)
        self.intra = sb("intra", [128, 8, 128], F32)
        self.kdec = sb("kdec", [128, 2, 8], F32)
        self.qdec = sb("qdec", [128, 4, 64], F32)
        self.sdec = sb("sdec", [128, 8], F32)
        self.S32 = sb("S32", [128, 1024], F32)
        self.Sbf = sb("Sbf", [128, 5, 1024], BF16)
        self.logit = sb("logit", [128, NT, 36], F32)
        self.comb = sb("comb", [128, NT, 32], F32)
        self.wr_f = sb("wr_f", [128, 8, 36], F32)
        self.rb = sb("rb", [128, 36], F32)
        self.small = sb("small", [128, 256], F32)
        self.P = st.enter_context(nc.psum_tensor("P", [128, 8, 512], F32))
        self.h_tm = self.BIG[:].rearrange("p (t d) -> p t d", t=NT)
        self.wslot_n = 0
        self.eps_ln = None

    def dump(self, name, ap, shape, reads):
        dd = DEBUG["dumps"]
        if not dd or name not in dd or name in self.dumps:
            return
        t = self.nc.dram_tensor("dbg_" + name, list(shape), ap.dtype, kind="ExternalOutput").ap()
        self.dumps[name] = t
        self.S.dma("sp", lambda e: e.dma_start(out=t, in_=ap), reads=reads, key="dbg_" + name)

    def ps(self, b):
        return self.P[:, b, :]

    def next_slot(self):
        s = self.wslot_n % 6
        self.wslot_n += 1
        return s

    def load_w(self, src2d, kc, ncols, slot=None, col0=0):
        if slot is None:
            slot = self.next_slot()
        dst = self.wsl[:, slot, 0:kc, col0:col0 + ncols]
        src = src2d.rearrange("(c p) n -> p c n", p=128)
        self.S.dma("pool", lambda e: e.dma_start(out=dst, in_=src), writes=[f"w{slot}"], key=f"w{slot}_{col0}", nobar=True)
        return slot

    def consts(self):
        S, dr = self.S, self.dr
        for nm, dst in (("c_ident", self.ident_f), ("c_ones", self.ones_f), ("c_intra", self.intra), ("c_kdec", self.kdec),
                        ("c_qdec", self.qdec), ("c_sdec", self.sdec)):
            S.dma("sp", lambda e, d=dst, s=dr[nm]: e.dma_start(out=d[:], in_=s), writes=[nm])
        S.dve(lambda e: e.tensor_copy(out=self.ident_b[:], in_=self.ident_f[:]), reads=["c_ident"], writes=["ident_b"])

    def ln_tile(self, x, R_x, T, pb, router, make_hT):
        S = self.S
        sm = self.small
        st_ap = sm[:, pb * 16:pb * 16 + 12]
        mv = sm[:, 32 + pb * 4:32 + pb * 4 + 2]
        rstd = sm[:, 40 + pb:41 + pb]
        gbb = self.PH[:, 0:2048].rearrange("p (a d) -> p a d", a=2)
        for hh in range(2):
            S.dve(lambda e, hh=hh: e.bn_stats(out=st_ap[:, hh * 6:(hh + 1) * 6], in_=x[:, hh * 512:(hh + 1) * 512]), reads=[R_x], writes=[f"lnst{pb}"])
        S.dve(lambda e: e.bn_aggr(out=mv, in_=st_ap.rearrange("p (a b) -> p a b", b=6)), reads=[f"lnst{pb}"], writes=[f"lnmv{pb}"])
        S.act(lambda e: e.activation(out=rstd, in_=mv[:, 1:2], func=AF.Sqrt, bias=LN_EPS, scale=1.0), reads=[f"lnmv{pb}"], writes=[f"lnrs{pb}"])
        S.dve(lambda e: e.reciprocal(out=rstd, in_=rstd), reads=[f"lnrs{pb}"], writes=[f"lnrs{pb}"])
        S.dve(lambda e: e.scalar_tensor_tensor(out=x, in0=x, scalar=mv[:, 0:1], in1=gbb[:, 0, :], op0=ALU.subtract, op1=ALU.mult),
              reads=[R_x, f"lnmv{pb}", "lngb"], writes=[R_x])
        S.dve(lambda e: e.scalar_tensor_tensor(out=x, in0=x, scalar=rstd, in1=gbb[:, 1, :], op0=ALU.mult, op1=ALU.add),
              reads=[R_x, f"lnrs{pb}", "lngb"], writes=[R_x])
        if not make_hT:
            return
        for c in range(8):
            b = 2 * pb + c // 4
            S.pe(lambda e, c=c, b=b: e.transpose(out=self.P[:, b, (c % 4) * 128:(c % 4 + 1) * 128], in_=x[:, c * 128:(c + 1) * 128], identity=self.ident_f[:]),
                 reads=[R_x, "c_ident"], writes=[f"ps{b}"])
        tsl = slice(T * 128, (T + 1) * 128)
        if not router:
            for k in range(2):
                b = 2 * pb + k
                S.act(lambda e, k=k, b=b: e.activation(out=self.hT[:, 4 * k:4 * k + 4, tsl], in_=self.P[:, b, :].rearrange("p (c t) -> p c t", c=4), func=AF.Copy),
                      reads=[], writes=[f"ps{b}", f"hT{T}"])
        else:
            hTf = self.PH[:, 2048 + pb * 1024:2048 + (pb + 1) * 1024].rearrange("p (c t) -> p c t", c=8)
            for k in range(2):
                b = 2 * pb + k
                S.act(lambda e, k=k, b=b: e.activation(out=hTf[:, 4 * k:4 * k + 4, :], in_=self.P[:, b, :].rearrange("p (c t) -> p c t", c=4), func=AF.Copy),
                      reads=[], writes=[f"ps{b}", f"hTf{pb}"])
            S.dve(lambda e: e.tensor_copy(out=self.hT[:, :, tsl], in_=hTf), reads=[f"hTf{pb}"], writes=[f"hT{T}"])
            bl = 4 + pb
            for c in range(8):
                S.pe(lambda e, c=c: e.matmul(self.P[:, bl, 0:36], lhsT=hTf[:, c, :], rhs=self.wr_f[:, c, :], start=(c == 0), stop=(c == 7)),
                     reads=[f"hTf{pb}", "wr_f"], writes=[f"ps{bl}"])
            S.dve(lambda e: e.tensor_tensor(out=self.logit[:, T, :], in0=self.P[:, bl, 0:36], in1=self.rb[:], op=ALU.add),
                  reads=["rb"], writes=[f"ps{bl}", f"logit{T}"])

    def load_ln_params(self, idx):
        gbb = self.PH[:, 0:2048].rearrange("p (a d) -> p a d", a=2)
        for a in range(2):
            self.S.dma("sp", lambda e, a=a: e.dma_start(out=gbb[:, a, :], in_=self.dr["ln_gb"][idx, a].partition_broadcast(128)),
                       writes=["lngb"], key=f"lngb{a}")

    def phase_in(self):
        S = self.S
        self.load_ln_params(0)
        for T in range(NT):
            S.dma("sp", lambda e, T=T: e.dma_start(out=self.h_tm[:, T, :], in_=self.x[T * 128:(T + 1) * 128, :]), writes=[f"htm{T}"], key=f"ldx{T}")
        for T in range(NT):
            self.ln_tile(self.h_tm[:, T, :], f"htm{T}", T, T % 2, router=False, make_hT=True)
            S.dma("sp", lambda e, T=T: e.dma_start(out=self.spillA[T * 128:(T + 1) * 128, :], in_=self.h_tm[:, T, :]), reads=[f"htm{T}"], key=f"sp{T % 4}")
        self.dump("h0_tm", self.h_tm, [128, NT, D], [f"htm{T}" for T in range(NT)])
        self.dump("h0_T", self.hT[:], [128, 8, SEQ], [f"hT{T}" for T in range(NT)])


    def abar_tables(self, lre, lim, lst, tmp, tag):
        S = self.S
        R = lambda k: f"{tag}_t{k}"
        t = tmp
        S.act(lambda e: e.activation(out=lst, in_=lst, func=AF.Exp), reads=[f"{tag}_lst"], writes=[f"{tag}_lst"])
        S.dve(lambda e: e.tensor_scalar(out=lre, in0=lre, scalar1=-1e-4, scalar2=None, op0=ALU.min), reads=[f"{tag}_lre"], writes=[f"{tag}_lre"])
        S.dve(lambda e: e.tensor_tensor(out=t[2], in0=lre, in1=lst, op=ALU.mult), reads=[f"{tag}_lre", f"{tag}_lst"], writes=[R(2)])
        S.act(lambda e: e.activation(out=t[2], in_=t[2], func=AF.Exp), reads=[R(2)], writes=[R(2)])
        S.dve(lambda e: e.tensor_tensor(out=t[3], in0=lim, in1=lst, op=ALU.mult), reads=[f"{tag}_lim", f"{tag}_lst"], writes=[R(3)])
        S.dve(lambda e: e.tensor_scalar(out=t[4], in0=t[3], scalar1=1.0 / TWO_PI, scalar2=None, op0=ALU.mult), reads=[R(3)], writes=[R(4)])
        t5i = t[5].bitcast(I32)
        S.dve(lambda e: e.tensor_copy(out=t5i, in_=t[4]), reads=[R(4)], writes=[R(5)])
        S.dve(lambda e: e.tensor_copy(out=t[4], in_=t5i), reads=[R(5)], writes=[R(4)])
        S.dve(lambda e: e.scalar_tensor_tensor(out=t[3], in0=t[4], scalar=-TWO_PI, in1=t[3], op0=ALU.mult, op1=ALU.add), reads=[R(4), R(3)], writes=[R(3)])
        def wrap(x, k):
            S.dve(lambda e: e.tensor_scalar(out=t[k], in0=x, scalar1=math.pi, scalar2=-TWO_PI, op0=ALU.is_gt, op1=ALU.mult), reads=[R(3), R(4)], writes=[R(k)])
            S.dve(lambda e: e.tensor_tensor(out=x, in0=x, in1=t[k], op=ALU.add), reads=[R(3), R(4), R(k)], writes=[R(3), R(4)])
            S.dve(lambda e: e.tensor_scalar(out=t[k], in0=x, scalar1=-math.pi, scalar2=TWO_PI, op0=ALU.is_lt, op1=ALU.mult), reads=[R(3), R(4)], writes=[R(k)])
            S.dve(lambda e: e.tensor_tensor(out=x, in0=x, in1=t[k], op=ALU.add), reads=[R(3), R(4), R(k)], writes=[R(3), R(4)])
        wrap(t[3], 5)
        S.dve(lambda e: e.tensor_scalar(out=t[4], in0=t[3], scalar1=math.pi / 2, scalar2=None, op0=ALU.add), reads=[R(3)], writes=[R(4)])
        wrap(t[4], 5)
        S.act(lambda e: e.activation(out=t[3], in_=t[3], func=AF.Sin), reads=[R(3)], writes=[R(3)])
        S.act(lambda e: e.activation(out=t[4], in_=t[4], func=AF.Sin), reads=[R(4)], writes=[R(4)])
        S.dve(lambda e: e.tensor_tensor(out=t[0], in0=t[2], in1=t[4], op=ALU.mult), reads=[R(2), R(4)], writes=[R(0)])
        S.dve(lambda e: e.tensor_tensor(out=t[1], in0=t[2], in1=t[3], op=ALU.mult), reads=[R(2), R(3)], writes=[R(1)])
        return t[0], t[1]

    def cmul(self, ore, oim, are, aim, bre, bim, t, tag):
        S = self.S
        S.dve(lambda e: e.tensor_tensor(out=t[0], in0=are, in1=bre, op=ALU.mult), reads=[tag], writes=[tag + "x0"])
        S.dve(lambda e: e.tensor_tensor(out=t[1], in0=aim, in1=bim, op=ALU.mult), reads=[tag], writes=[tag + "x1"])
        S.dve(lambda e: e.tensor_tensor(out=ore, in0=t[0], in1=t[1], op=ALU.subtract), reads=[tag + "x0", tag + "x1"], writes=[tag])
        S.dve(lambda e: e.tensor_tensor(out=t[0], in0=are, in1=bim, op=ALU.mult), reads=[tag], writes=[tag + "x0"])
        S.dve(lambda e: e.tensor_tensor(out=t[1], in0=aim, in1=bre, op=ALU.mult), reads=[tag], writes=[tag + "x1"])
        S.dve(lambda e: e.tensor_tensor(out=oim, in0=t[0], in1=t[1], op=ALU.add), reads=[tag + "x0", tag + "x1"], writes=[tag])

    def phase_s5(self, l):
        S, dr = self.S, self.dr
        BIG, PH, Y16, uz = self.BIG, self.PH, self.Y16, self.uz
        W32 = self.wsl[:].rearrange("p s c n -> p (s c n)").bitcast(F32)
        T8f = self.T8[:].rearrange("p a f -> p (a f)")
        SA = [[BIG[:, 0:2048], BIG[:, 2048:4096]], [BIG[:, 4096:6144], BIG[:, 6144:8192]]]
        SB = [[BIG[:, 4096:6144], BIG[:, 6144:8192]], [W32[:, 4096:6144], T8f]]
        hb = BIG[:, 8192:12288].bitcast(BF16).rearrange("p (b c t) -> p b c t", b=2, c=2)
        tp = [BIG[:, 12288 + k * 512:12288 + (k + 1) * 512] for k in range(8)]
        tq = [BIG[:, 8192 + k * 512:8192 + (k + 1) * 512] for k in range(8)]
        T8f_ = self.T8[:].rearrange("p a f -> p (a f)")
        def zbuf(base_ap, k):
            return base_ap[:, k * 384:(k + 1) * 384]
        ZB = [[[zbuf(PH[:, 2048:4096], 0), zbuf(PH[:, 2048:4096], 1)], [zbuf(PH[:, 2048:4096], 2), zbuf(PH[:, 2048:4096], 3)]],
              [[zbuf(T8f_, 0), zbuf(T8f_, 1)], [zbuf(T8f_, 2), zbuf(T8f_, 3)]]]
        SSM = [[[b[:, 128:384] for b in pair] for pair in st_] for st_ in ZB]
        coef = PH[:, 1024:1744].rearrange("p (k c j) -> p k c j", k=15, c=3)
        lamp = PH[:, 1744:1792].rearrange("p (r j) -> p r j", r=3)
        tiny = [PH[:, 1792 + 16 * k:1808 + 16 * k] for k in range(12)]
        dcol = PH[:, 1984:1988]
        BBs = [Y16[:, 0:2048].rearrange("p (j m) -> p j m", j=16), Y16[:, 2048:4096].rearrange("p (j m) -> p j m", j=16)]
        CCb = [Y16[:, 4096:6144].rearrange("p (j m) -> p j m", j=16), Y16[:, 6144:8192].rearrange("p (j m) -> p j m", j=16)]
        slots = [self.load_w(dr["w_in"][l, :, UOFF + 256 * b:UOFF + 256 * (b + 1)], 8, 256) for b in range(2)]
        bank = 0
        for b in range(2):
            for tt in range(4):
                for o2 in range(2):
                    oc = 2 * b + o2
                    bk = bank % 8
                    bank += 1
                    for c in range(8):
                        S.pe(lambda e, c=c, bk=bk, b=b, o2=o2, tt=tt: e.matmul(self.P[:, bk, :], lhsT=self.wsl[:, slots[b], c, o2 * 128:(o2 + 1) * 128],
                                                                          rhs=self.hT[:, c, tt * 512:(tt + 1) * 512], start=(c == 0), stop=(c == 7)),
                             reads=[f"w{slots[b]}"] + [f"hT{4 * tt + k}" for k in range(4)], writes=[f"ps{bk}"])
                    S.act(lambda e, bk=bk, oc=oc, tt=tt: e.activation(out=uz[:, oc, tt * 512:(tt + 1) * 512], in_=self.P[:, bk, :], func=AF.Copy),
                          writes=[f"ps{bk}", f"uz{oc}_{tt}"])
        self.dump("uT", uz[:], [128, 4, SEQ], [f"uz{oc}_{tt}" for oc in range(4) for tt in range(4)])
        S.dma("sp", lambda e: e.dma_start(out=lamp, in_=dr["s5_lamp"][l]), writes=["pl_lre", "pl_lim", "pl_lst"], key="lamp")
        S.dma("sp", lambda e: e.dma_start(out=dcol, in_=dr["s5_dcol"][l]), writes=["dcol"], key="dcol")
        ccf = [BIG[:, 12288:14336].rearrange("p (j m) -> p j m", j=16), BIG[:, 14336:16384].rearrange("p (j m) -> p j m", j=16)]
        for comp in range(2):
            S.dma("pool", lambda e, comp=comp: e.dma_start(out=BBs[comp], in_=dr["s5_BB"][l, comp]), writes=[f"BBs{comp}"], key=f"BBs{comp}")
            S.dma("sp", lambda e, comp=comp: e.dma_start(out=ccf[comp], in_=dr["s5_CC"][l, comp]), writes=[f"ccf{comp}"], key=f"ccf{comp}")
        are, aim = self.abar_tables(lamp[:, 0, :], lamp[:, 1, :], lamp[:, 2, :], tiny[0:6], "pl")
        pw = {}
        S.dve(lambda e: e.tensor_copy(out=coef[:, 0, 0, :], in_=are), reads=["pl_t0"], writes=["pw"])
        S.dve(lambda e: e.tensor_copy(out=coef[:, 0, 1, :], in_=aim), reads=["pl_t1"], writes=["pw"])
        for idx in range(1, 15):
            if POWS[idx] <= 8:
                a_i, b_i = idx - 1, 0
            else:
                a_i, b_i = idx - 1, idx - 1
            self.cmul(coef[:, idx, 0, :], coef[:, idx, 1, :], coef[:, a_i, 0, :], coef[:, a_i, 1, :], coef[:, b_i, 0, :], coef[:, b_i, 1, :], tiny[6:8], "pw")
        for idx in range(15):
            S.dve(lambda e, idx=idx: e.tensor_scalar(out=coef[:, idx, 2, :], in0=coef[:, idx, 1, :], scalar1=-1.0, scalar2=None, op0=ALU.mult), reads=["pw"], writes=["pw"])
        lr, li = lamp[:, 0, :], lamp[:, 1, :]
        z = tiny[8:12]
        S.dve(lambda e: e.tensor_tensor(out=z[0], in0=lr, in1=lr, op=ALU.mult), reads=["pl_lre"], writes=["z0"])
        S.dve(lambda e: e.tensor_tensor(out=z[1], in0=li, in1=li, op=ALU.mult), reads=["pl_lim"], writes=["z1"])
        S.dve(lambda e: e.tensor_tensor(out=z[0], in0=z[0], in1=z[1], op=ALU.add), reads=["z0", "z1"], writes=["z0"])
        S.dve(lambda e: e.reciprocal(out=z[0], in_=z[0]), reads=["z0"], writes=["z0"])
        S.dve(lambda e: e.tensor_scalar(out=z[1], in0=are, scalar1=-1.0, scalar2=None, op0=ALU.add), reads=["pl_t0", "z0"], writes=["z1"])
        S.dve(lambda e: e.tensor_tensor(out=z[2], in0=z[1], in1=lr, op=ALU.mult), reads=["z1", "pl_lre"], writes=["z2"])
        S.dve(lambda e: e.tensor_tensor(out=z[3], in0=aim, in1=li, op=ALU.mult), reads=["pl_t1", "pl_lim"], writes=["z3"])
        S.dve(lambda e: e.tensor_tensor(out=z[2], in0=z[2], in1=z[3], op=ALU.add), reads=["z2", "z3"], writes=["z2"])
        S.dve(lambda e: e.tensor_tensor(out=z[2], in0=z[2], in1=z[0], op=ALU.mult), reads=["z2", "z0"], writes=["z2"])
        S.dve(lambda e: e.tensor_tensor(out=z[3], in0=aim, in1=lr, op=ALU.mult), reads=["pl_t1", "pl_lre", "z2"], writes=["z3"])
        S.dve(lambda e: e.tensor_tensor(out=z[1], in0=z[1], in1=li, op=ALU.mult), reads=["z1", "pl_lim"], writes=["z1"])
        S.dve(lambda e: e.tensor_tensor(out=z[3], in0=z[3], in1=z[1], op=ALU.subtract), reads=["z3", "z1"], writes=["z3"])
        S.dve(lambda e: e.tensor_tensor(out=z[3], in0=z[3], in1=z[0], op=ALU.mult), reads=["z3", "z0"], writes=["z3"])
        S.dve(lambda e: e.tensor_scalar(out=z[1], in0=z[3], scalar1=-1.0, scalar2=None, op0=ALU.mult), reads=["z3"], writes=["z1"])
        tmpc = self.T8[:].rearrange("p a f -> p (a f)")[:, 0:128]
        for j in range(16):
            zr, zi, nzi = z[2][:, j:j + 1], z[3][:, j:j + 1], z[1][:, j:j + 1]
            S.dve(lambda e, j=j, zr=zr: e.tensor_scalar(out=tmpc, in0=ccf[0][:, j, :], scalar1=zr, scalar2=None, op0=ALU.mult), reads=["ccf0", "z2"], writes=["tmpc"])
            S.dve(lambda e, j=j, nzi=nzi: e.scalar_tensor_tensor(out=CCb[0][:, j, :], in0=ccf[1][:, j, :], scalar=nzi, in1=tmpc, op0=ALU.mult, op1=ALU.add), reads=["ccf1", "z1", "tmpc"], writes=["CC0"])
            S.dve(lambda e, j=j, zi=zi: e.tensor_scalar(out=tmpc, in0=ccf[0][:, j, :], scalar1=zi, scalar2=None, op0=ALU.mult), reads=["ccf0", "z3", "CC0"], writes=["tmpc"])
            S.dve(lambda e, j=j, zr=zr: e.scalar_tensor_tensor(out=CCb[1][:, j, :], in0=ccf[1][:, j, :], scalar=zr, in1=tmpc, op0=ALU.mult, op1=ALU.add), reads=["ccf1", "z2", "tmpc"], writes=["CC1"])
        S.barrier()
        S.dve(lambda e: e.memset(PH[:, 2048:2048 + 1536], 0.0), writes=["zpad0"])
        S.dve(lambda e: e.memset(T8f_[:, 0:1536], 0.0), writes=["zpad1"])
        v8 = lambda ap: ap.rearrange("p (b s) -> p b s", s=8)
        t8 = lambda ap: ap.rearrange("p (s b) -> p s b", s=8)
        nbank = [0]
        l3t = [PH[:, 3072 + 256 * k:3072 + 256 * (k + 1)] for k in range(4)]

        def stageX(j):
            q, sx = j // 4, j % 2
            A, Ssm = SA[sx], SSM[sx]
            cn = f"sA{sx}_"
            for tt in range(4):
                for comp in range(2):
                    bk = 4 + nbank[0] % 4
                    nbank[0] += 1
                    S.pe(lambda e, bk=bk, comp=comp, tt=tt: e.matmul(self.P[:, bk, :], lhsT=BBs[comp][:, j, :], rhs=uz[:, q, tt * 512:(tt + 1) * 512], start=True, stop=True),
                         reads=[f"BBs{comp}", f"uz{q}_{tt}"], writes=[f"ps{bk}"])
                    S.act(lambda e, bk=bk, comp=comp, tt=tt: e.activation(out=t8(A[comp])[:, :, tt * 64:(tt + 1) * 64].rearrange("p s b -> p b s"), in_=self.P[:, bk, :].rearrange("p (b s) -> p b s", s=8), func=AF.Copy),
                          writes=[f"ps{bk}", f"{cn}{comp}"])

        def stageXd(j):
            q, sx = j // 4, j % 2
            A, Ssm = SA[sx], SSM[sx]
            cn = f"sA{sx}_"
            idx1 = POWS.index(1)
            car, cai, cnai = coef[:, idx1, 0, j:j + 1], coef[:, idx1, 1, j:j + 1], coef[:, idx1, 2, j:j + 1]
            a_re, a_im = t8(A[0]), t8(A[1])
            for tau in range(1, 8):
                S.dve(lambda e, tau=tau: e.scalar_tensor_tensor(out=a_re[:, tau, :], in0=a_re[:, tau - 1, :], scalar=car, in1=a_re[:, tau, :], op0=ALU.mult, op1=ALU.add),
                      reads=[f"{cn}0", "pw"], writes=[f"{cn}0"])
                S.dve(lambda e, tau=tau: e.scalar_tensor_tensor(out=a_im[:, tau, :], in0=a_im[:, tau - 1, :], scalar=car, in1=a_im[:, tau, :], op0=ALU.mult, op1=ALU.add),
                      reads=[f"{cn}1", "pw"], writes=[f"{cn}1"])
                S.dve(lambda e, tau=tau: e.scalar_tensor_tensor(out=a_re[:, tau, :], in0=a_im[:, tau - 1, :], scalar=cnai, in1=a_re[:, tau, :], op0=ALU.mult, op1=ALU.add),
                      reads=[f"{cn}0", f"{cn}1", "pw"], writes=[f"{cn}0"])
                S.dve(lambda e, tau=tau: e.scalar_tensor_tensor(out=a_im[:, tau, :], in0=a_re[:, tau - 1, :], scalar=cai, in1=a_im[:, tau, :], op0=ALU.mult, op1=ALU.add),
                      reads=[f"{cn}0", f"{cn}1", "pw"], writes=[f"{cn}1"])
            s0, s1, n0, n1 = Ssm[0], Ssm[1], f"sS0{sx}_", f"sS1{sx}_"
            for comp in range(2):
                S.act(lambda e, comp=comp, s0=s0: e.activation(out=s0[comp], in_=t8(A[comp])[:, 7, :], func=AF.Copy),
                      reads=[f"{cn}{comp}", f"zpad{sx}"], writes=[f"{n0}{comp}"])
            zb0, zb1 = ZB[sx][0], ZB[sx][1]
            for d in (1, 2, 4, 8, 16, 32, 64, 128):
                idx = POWS.index(8 * d)
                c_r, c_i, c_ni = coef[:, idx, 0, j:j + 1], coef[:, idx, 1, j:j + 1], coef[:, idx, 2, j:j + 1]
                sh = slice(128 - d, 384 - d)
                S.dve(lambda e, zb0=zb0, s0=s0, s1=s1, sh=sh, c_r=c_r: e.scalar_tensor_tensor(out=s1[0], in0=zb0[0][:, sh], scalar=c_r, in1=s0[0], op0=ALU.mult, op1=ALU.add),
                      reads=[f"{n0}0", "pw", f"zpad{sx}"], writes=[f"{n1}0"])
                S.dve(lambda e, zb0=zb0, s0=s0, s1=s1, sh=sh, c_r=c_r: e.scalar_tensor_tensor(out=s1[1], in0=zb0[1][:, sh], scalar=c_r, in1=s0[1], op0=ALU.mult, op1=ALU.add),
                      reads=[f"{n0}1", "pw", f"zpad{sx}"], writes=[f"{n1}1"])
                S.dve(lambda e, zb0=zb0, s1=s1, sh=sh, c_ni=c_ni: e.scalar_tensor_tensor(out=s1[0], in0=zb0[1][:, sh], scalar=c_ni, in1=s1[0], op0=ALU.mult, op1=ALU.add),
                      reads=[f"{n0}1", f"{n1}0", "pw"], writes=[f"{n1}0"])
                S.dve(lambda e, zb0=zb0, s1=s1, sh=sh, c_i=c_i: e.scalar_tensor_tensor(out=s1[1], in0=zb0[0][:, sh], scalar=c_i, in1=s1[1], op0=ALU.mult, op1=ALU.add),
                      reads=[f"{n0}0", f"{n1}1", "pw"], writes=[f"{n1}1"])
                s0, s1, n0, n1 = s1, s0, n1, n0
                zb0, zb1 = zb1, zb0

        def stageY(j):
            q, sx, hbuf = j // 4, j % 2, j % 2
            A, Ssm = SA[sx], SSM[sx]
            cn = f"sA{sx}_"
            s0, n0 = Ssm[0], f"sS0{sx}_"
            rr = [f"{n0}0", f"{n0}1", "pw"]
            for tau in range(8):
                car, cai, cnai = coef[:, tau, 0, j:j + 1], coef[:, tau, 1, j:j + 1], coef[:, tau, 2, j:j + 1]
                dre, dim = t8(A[0])[:, tau, 1:], t8(A[1])[:, tau, 1:]
                S.dve(lambda e, dre=dre, car=car: e.scalar_tensor_tensor(out=dre, in0=s0[0][:, 0:255], scalar=car, in1=dre, op0=ALU.mult, op1=ALU.add), reads=rr + [f"{cn}0"], writes=[f"{cn}0"])
                S.dve(lambda e, dim=dim, car=car: e.scalar_tensor_tensor(out=dim, in0=s0[1][:, 0:255], scalar=car, in1=dim, op0=ALU.mult, op1=ALU.add), reads=rr + [f"{cn}1"], writes=[f"{cn}1"])
                S.dve(lambda e, dre=dre, cnai=cnai: e.scalar_tensor_tensor(out=dre, in0=s0[1][:, 0:255], scalar=cnai, in1=dre, op0=ALU.mult, op1=ALU.add), reads=rr + [f"{cn}0"], writes=[f"{cn}0"])
                S.dve(lambda e, dim=dim, cai=cai: e.scalar_tensor_tensor(out=dim, in0=s0[0][:, 0:255], scalar=cai, in1=dim, op0=ALU.mult, op1=ALU.add), reads=rr + [f"{cn}1"], writes=[f"{cn}1"])
            S.act(lambda e: e.activation(out=hb[:, hbuf, 0, :].rearrange("p (b s) -> p b s", s=8), in_=t8(A[0]).rearrange("p s b -> p b s"), func=AF.Copy), reads=[f"{cn}0"], writes=[f"hb{hbuf}"])
            S.act(lambda e: e.activation(out=hb[:, hbuf, 1, :].rearrange("p (b s) -> p b s", s=8), in_=t8(A[1]).rearrange("p s b -> p b s"), func=AF.Identity, scale=-1.0), reads=[f"{cn}1"], writes=[f"hb{hbuf}"])
            for tt in range(4):
                for comp in range(2):
                    S.pe(lambda e, tt=tt, comp=comp: e.matmul(self.P[:, tt, :], lhsT=CCb[comp][:, j, :], rhs=hb[:, hbuf, comp, tt * 512:(tt + 1) * 512],
                                                              start=(j == 4 * q and comp == 0), stop=(j == 4 * q + 3 and comp == 1)),
                         reads=[f"CC{comp}", f"hb{hbuf}"], writes=[f"ps{tt}"])
            if j % 4 != 3:
                return
            for tt in range(4):
                ya, yb = tp[2 * (tt % 2)], tp[2 * (tt % 2) + 1]
                na, nb = f"gl{2 * (tt % 2)}", f"gl{2 * (tt % 2) + 1}"
                usl = uz[:, q, tt * 512:(tt + 1) * 512]
                S.dve(lambda e, ya=ya, usl=usl, tt=tt: e.scalar_tensor_tensor(out=ya, in0=usl, scalar=dcol[:, q:q + 1], in1=self.P[:, tt, :], op0=ALU.mult, op1=ALU.add),
                      reads=[f"uz{q}_{tt}", "dcol"], writes=[f"ps{tt}", na])
                S.act(lambda e, ya=ya, yb=yb: e.activation(out=yb, in_=ya, func=AF.Square), reads=[na], writes=[nb])
                S.dve(lambda e, yb=yb: e.tensor_scalar(out=yb, in0=yb, scalar1=0.044715, scalar2=1.0, op0=ALU.mult, op1=ALU.add), reads=[nb], writes=[nb])
                S.dve(lambda e, ya=ya, yb=yb: e.tensor_tensor(out=yb, in0=yb, in1=ya, op=ALU.mult), reads=[na, nb], writes=[nb])
                S.act(lambda e, yb=yb: e.activation(out=yb, in_=yb, func=AF.Sigmoid, scale=1.5957691216057308), reads=[nb], writes=[nb])
                S.dve(lambda e, ya=ya, yb=yb, usl=usl: e.tensor_tensor(out=usl, in0=ya, in1=yb, op=ALU.mult), reads=[na, nb], writes=[f"uz{q}_{tt}"])

        stageX(0)
        stageXd(0)
        for j in range(16):
            if j + 1 < 16:
                stageX(j + 1)
            stageY(j)
            if j + 1 < 16:
                stageXd(j + 1)
        self.dump("zT", uz[:], [128, 4, SEQ], [f"uz{oc}_{tt}" for oc in range(4) for tt in range(4)])
        S.barrier()
        gs = [self.load_w(dr["w_glu"][l, :, 256 * b:256 * (b + 1)], 4, 256) for b in range(2)]
        for tt in range(4):
            for oc in range(4):
                bk = 4 * (tt % 2) + oc
                for kc in range(4):
                    S.pe(lambda e, bk=bk, oc=oc, kc=kc, tt=tt: e.matmul(self.P[:, bk, :], lhsT=self.wsl[:, gs[oc // 2], kc, (oc % 2) * 128:(oc % 2 + 1) * 128],
                                                                    rhs=uz[:, kc, tt * 512:(tt + 1) * 512], start=(kc == 0), stop=(kc == 3)),
                         reads=[f"w{gs[oc // 2]}"] + [f"uz{k}_{tt}" for k in range(4)], writes=[f"ps{bk}"])
            for oc in range(4):
                bk = 4 * (tt % 2) + oc
                sg = self.T8[:, oc, :]
                S.act(lambda e, bk=bk, sg=sg: e.activation(out=sg, in_=self.P[:, bk, :], func=AF.Sigmoid), writes=[f"ps{bk}", f"T8_{oc}"])
                S.dve(lambda e, sg=sg, oc=oc, tt=tt: e.tensor_tensor(out=uz[:, oc, tt * 512:(tt + 1) * 512], in0=uz[:, oc, tt * 512:(tt + 1) * 512], in1=sg, op=ALU.mult),
                      reads=[f"T8_{oc}", f"uz{oc}_{tt}"], writes=[f"uz{oc}_{tt}"])
        self.dump("z2T", uz[:], [128, 4, SEQ], [f"uz{oc}_{tt}" for oc in range(4) for tt in range(4)])


    def big_views(self):
        Bb = self.BIG[:].bitcast(BF16)
        v = {}
        v["qT"] = Bb[:, 0:4096].rearrange("p (c t) -> p c t", c=4)
        v["kT"] = Bb[:, 4096:8192].rearrange("p (c t) -> p c t", c=4)
        v["qdT"] = Bb[:, 8192:12288].rearrange("p (c t) -> p c t", c=4)
        v["v_tm"] = Bb[:, 12288:20480].rearrange("p (t d) -> p t d", t=8)
        v["rT"] = Bb[:, 20480:28672].rearrange("p (c t) -> p c t", c=8)
        v["mergedT"] = Bb[:, 0:8192].rearrange("p (c t) -> p c t", c=8)
        v["rt"] = [self.BIG[:, 14336 + k * 512:14336 + (k + 1) * 512] for k in range(4)]
        return v

    def wbig(self, k):
        return self.wsl[:, 2 * k:2 * k + 2, :, :].rearrange("p s c n -> p (s c n)").rearrange("p (c n) -> p c n", c=8)

    def load_wbig(self, src2d, kc, k):
        dst = self.wbig(k)[:, 0:kc, :]
        src = src2d.rearrange("(c p) n -> p c n", p=128)
        self.S.dma("pool", lambda e: e.dma_start(out=dst, in_=src), writes=[f"w{2 * k}", f"w{2 * k + 1}"], key=f"wb{k}", nobar=True)

    def phase_ret(self, l, half):
        S, dr, P = self.S, self.dr, self.P
        V = self.big_views()
        qT, kT, qdT, v_tm, rT, rt = V["qT"], V["kT"], V["qdT"], V["v_tm"], V["rT"], V["rt"]
        PH, T8 = self.PH, self.T8
        rope = PH[:, 0:2048].rearrange("p (a t) -> p a t", a=2)
        ktm = PH[:, 2048:3072].bitcast(BF16).rearrange("p (b e f) -> p b e f", b=2, e=2)
        ST = PH[:, 3072:3584].bitcast(BF16).rearrange("p (h i) -> p h i", h=8)
        sgt = PH[:, 3584:4096].bitcast(BF16).rearrange("p (b f) -> p b f", b=2)
        tok0 = half * 1024
        S32, Sbf = self.S32, self.Sbf
        if half == 0:
            S.pool(lambda e: e.memset(S32[:], 0.0), writes=["S32"])
            S.pool(lambda e: e.memset(Sbf[:, 0, :], 0.0), writes=["Sbf0"])
        for a in range(2):
            S.dma("sp", lambda e, a=a: e.dma_start(out=rope[:, a, :], in_=dr["c_rope"][a, :, tok0:tok0 + 1024]), writes=[f"rope{a}"])
        nb = 0
        for which, off, swoff, dst, nm in (("q", QOFF, 0, qT, "qT"), ("k", KOFF, 512, kT, "kT")):
            for b in range(2):
                sm = self.load_w(dr["w_in"][l, :, off + 256 * b:off + 256 * (b + 1)], 8, 256)
                ss = self.load_w(dr["w_qksw"][l, :, swoff + 256 * b:swoff + 256 * (b + 1)], 8, 256)
                for tt in range(2):
                    for o2 in range(2):
                        oc = 2 * b + o2
                        ba, bb_ = 2 * (nb % 4), 2 * (nb % 4) + 1
                        nb += 1
                        hts = [f"hT{8 * half + 4 * tt + k}" for k in range(4)]
                        g0 = tok0 + tt * 512
                        for c in range(8):
                            S.pe(lambda e, c=c, ba=ba, sm=sm, o2=o2, g0=g0: e.matmul(P[:, ba, :], lhsT=self.wsl[:, sm, c, o2 * 128:(o2 + 1) * 128], rhs=self.hT[:, c, g0:g0 + 512], start=(c == 0), stop=(c == 7)),
                                 reads=[f"w{sm}"] + hts, writes=[f"ps{ba}"])
                        for c in range(8):
                            S.pe(lambda e, c=c, bb_=bb_, ss=ss, o2=o2, g0=g0: e.matmul(P[:, bb_, :], lhsT=self.wsl[:, ss, c, o2 * 128:(o2 + 1) * 128], rhs=self.hT[:, c, g0:g0 + 512], start=(c == 0), stop=(c == 7)),
                                 reads=[f"w{ss}"] + hts, writes=[f"ps{bb_}"])
                        lsl = slice(tt * 512, (tt + 1) * 512)
                        r0, r1 = rt[2 * (nb % 2)], rt[2 * (nb % 2) + 1]
                        n0_, n1_ = f"rt{2 * (nb % 2)}", f"rt{2 * (nb % 2) + 1}"
                        S.dve(lambda e, ba=ba, lsl=lsl, r0=r0: e.tensor_tensor(out=r0, in0=P[:, ba, :], in1=rope[:, 0, lsl], op=ALU.mult), reads=["rope0"], writes=[f"ps{ba}", n0_])
                        S.dve(lambda e, bb_=bb_, lsl=lsl, r1=r1: e.tensor_tensor(out=r1, in0=P[:, bb_, :], in1=rope[:, 1, lsl], op=ALU.mult), reads=["rope1"], writes=[f"ps{bb_}", n1_])
                        S.dve(lambda e, dst=dst, oc=oc, lsl=lsl, r0=r0, r1=r1: e.tensor_tensor(out=dst[:, oc, lsl], in0=r0, in1=r1, op=ALU.add), reads=[n0_, n1_], writes=[f"{nm}{oc}_{tt}"])
                        if which == "q":
                            S.dve(lambda e, oc=oc, lsl=lsl: e.tensor_tensor(out=qdT[:, oc, lsl].rearrange("p (n i) -> p n i", i=64), in0=qT[:, oc, lsl].rearrange("p (n i) -> p n i", i=64),
                                                                         in1=self.qdec[:, oc, :].unsqueeze(1).broadcast_to([128, 8, 64]), op=ALU.mult),
                                   reads=[f"qT{oc}_{tt}", "c_qdec"], writes=[f"qdT{oc}_{tt}"])
        self.dump("qT", qT, [128, 4, 1024], [f"qT{oc}_{tt}" for oc in range(4) for tt in range(2)])
        self.dump("kT", kT, [128, 4, 1024], [f"kT{oc}_{tt}" for oc in range(4) for tt in range(2)])
        if DEBUG["stop"] == "retqk":
            return
        for vb in range(2):
            self.load_wbig(dr["w_in"][l, :, VOFF + 512 * vb:VOFF + 512 * (vb + 1)], 8, vb)
        nb = 0
        for tl in range(8):
            T = 8 * half + tl
            for vb in range(2):
                bk = nb % 8
                nb += 1
                for c in range(8):
                    S.pe(lambda e, c=c, bk=bk, vb=vb, T=T: e.matmul(P[:, bk, :], lhsT=self.hT[:, c, T * 128:(T + 1) * 128], rhs=self.wbig(vb)[:, c, :], start=(c == 0), stop=(c == 7)),
                         reads=[f"w{2 * vb}", f"w{2 * vb + 1}", f"hT{T}"], writes=[f"ps{bk}"])
                S.act(lambda e, bk=bk, tl=tl, vb=vb: e.activation(out=v_tm[:, tl, vb * 512:(vb + 1) * 512], in_=P[:, bk, :], func=AF.Copy), writes=[f"ps{bk}", f"v{tl}"])
        self.dump("v_tm", v_tm, [128, 8, 1024], [f"v{tl}" for tl in range(8)])
        nb = 0
        for b in range(4):
            sg_ = self.load_w(dr["w_in"][l, :, GOFF + 256 * b:GOFF + 256 * (b + 1)], 8, 256)
            for tt in range(2):
                for o2 in range(2):
                    oc = 2 * b + o2
                    bk = nb % 8
                    nb += 1
                    g0 = tok0 + tt * 512
                    for c in range(8):
                        S.pe(lambda e, c=c, bk=bk, sg_=sg_, o2=o2, g0=g0: e.matmul(P[:, bk, :], lhsT=self.wsl[:, sg_, c, o2 * 128:(o2 + 1) * 128], rhs=self.hT[:, c, g0:g0 + 512], start=(c == 0), stop=(c == 7)),
                             reads=[f"w{sg_}"] + [f"hT{8 * half + 4 * tt + k}" for k in range(4)], writes=[f"ps{bk}"])
                    S.act(lambda e, bk=bk, oc=oc, tt=tt: e.activation(out=rT[:, oc, tt * 512:(tt + 1) * 512], in_=P[:, bk, :], func=AF.Silu), writes=[f"ps{bk}"] + [f"rT{4 * tt + k}" for k in range(4)])

        def stageA(tl):
            T = 8 * half + tl
            pb = tl % 2
            tsl = slice(tl * 128, (tl + 1) * 128)
            Pb0 = P[:, 0, :].bitcast(BF16)
            for c4 in range(4):
                S.pe(lambda e, c4=c4: e.transpose(out=Pb0[:, c4 * 128:(c4 + 1) * 128], in_=kT[:, c4, tsl], identity=self.ident_b[:]),
                     reads=[f"kT{c4}_{tl // 4}", "ident_b"], writes=["ps0"])
            for eo in range(2):
                S.dve(lambda e, eo=eo: e.tensor_tensor(out=ktm[:, pb, eo, :].rearrange("p (h d) -> p h d", h=8), in0=Pb0[:, 0:512].rearrange("p (h d) -> p h d", h=8),
                                                       in1=self.kdec[:, eo, :].unsqueeze(2).broadcast_to([128, 8, 64]), op=ALU.mult),
                      reads=["c_kdec"], writes=["ps0", f"ktm{pb}_{eo}"])
            for eo in range(2):
                for h in range(8):
                    c4 = h // 2
                    S.pe(lambda e, h=h, c4=c4, eo=eo: e.matmul(P[:, 1 + h // 4, (h % 4) * 128:(h % 4 + 1) * 128], lhsT=ktm[:, pb, eo, c4 * 128:(c4 + 1) * 128],
                                                              rhs=v_tm[:, tl, h * 128:(h + 1) * 128], start=True, stop=True),
                         reads=[f"ktm{pb}_{eo}", f"v{tl}"], writes=[f"ps{1 + h // 4}"])
                tmp = T8[:, 3, :]
                S.dve(lambda e: e.tensor_tensor(out=S32[:].rearrange("p (h e) -> p h e", h=8), in0=S32[:].rearrange("p (h e) -> p h e", h=8),
                                                 in1=self.sdec[:].unsqueeze(2).broadcast_to([128, 8, 128]), op=ALU.mult), reads=["S32", "c_sdec"], writes=["S32"])
                for k in range(2):
                    S.dve(lambda e, k=k: e.tensor_tensor(out=S32[:, k * 512:(k + 1) * 512], in0=S32[:, k * 512:(k + 1) * 512], in1=P[:, 1 + k, :], op=ALU.add),
                          reads=["S32"], writes=["S32", f"ps{1 + k}"])
                if eo == 0:
                    idx = 3 + T % 2
                else:
                    idx = (T + 1) % 3
                S.act(lambda e, idx=idx: e.activation(out=Sbf[:, idx, :], in_=S32[:], func=AF.Copy), reads=["S32"], writes=[f"Sbf{idx}"])

        STs = [ST, PH[:, 3584:4096].bitcast(BF16).rearrange("p (h i) -> p h i", h=8)]

        def stageBf(tl):
            tsl = slice(tl * 128, (tl + 1) * 128)
            STb = STs[tl % 2]
            ST4 = STb.rearrange("p (c hh) i -> p c hh i", hh=2)
            in4 = self.intra[:].rearrange("p (c hh) i -> p c hh i", hh=2)
            for h in range(8):
                c4, hh = h // 2, h % 2
                S.pe(lambda e, c4=c4, hh=hh: e.matmul(P[:, 3 + hh, c4 * 128:(c4 + 1) * 128], lhsT=kT[hh * 64:(hh + 1) * 64, c4, tsl], rhs=qT[hh * 64:(hh + 1) * 64, c4, tsl], start=True, stop=True),
                     reads=[f"kT{c4}_{tl // 4}", f"qT{c4}_{tl // 4}"], writes=[f"ps{3 + hh}"])
            for hh in range(2):
                S.dve(lambda e, hh=hh: e.tensor_tensor(out=ST4[:, :, hh, :], in0=P[:, 3 + hh, :].rearrange("p (c i) -> p c i", c=4), in1=in4[:, :, hh, :], op=ALU.mult),
                      reads=["c_intra"], writes=[f"ps{3 + hh}", f"ST{tl % 2}_{hh}"])

        def stageBb(tl):
            T = 8 * half + tl
            tsl = slice(tl * 128, (tl + 1) * 128)
            ie, io = T % 3, 3 + T % 2
            STb = STs[tl % 2]
            rT4 = rT.rearrange("p (c hh) t -> p c hh t", hh=2)
            for h in range(8):
                c4, hh = h // 2, h % 2
                ob = P[:, 5 + hh, c4 * 128:(c4 + 1) * 128]
                S.pe(lambda e, h=h, ob=ob: e.matmul(ob, lhsT=v_tm[:, tl, h * 128:(h + 1) * 128], rhs=STb[:, h, :], start=True, stop=False),
                     reads=[f"v{tl}", f"ST{tl % 2}_{hh}"], writes=[f"ps{5 + hh}"])
                S.pe(lambda e, h=h, ob=ob, c4=c4, hh=hh: e.matmul(ob[:, 0:64], lhsT=Sbf[hh * 64:(hh + 1) * 64, ie, h * 128:(h + 1) * 128], rhs=qdT[hh * 64:(hh + 1) * 64, c4, tl * 128:tl * 128 + 64], start=False, stop=False),
                     reads=[f"Sbf{ie}", f"qdT{c4}_{tl // 4}"], writes=[f"ps{5 + hh}"])
                S.pe(lambda e, h=h, ob=ob, c4=c4, hh=hh: e.matmul(ob[:, 64:128], lhsT=Sbf[hh * 64:(hh + 1) * 64, io, h * 128:(h + 1) * 128], rhs=qdT[hh * 64:(hh + 1) * 64, c4, tl * 128 + 64:tl * 128 + 128], start=False, stop=True),
                     reads=[f"Sbf{io}", f"qdT{c4}_{tl // 4}"], writes=[f"ps{5 + hh}"])
            for hh in range(2):
                bo, bm, bq = 5 + hh, 7, 5 + hh
                o_sb, osq, w2 = (T8[:, 0, :], T8[:, 1, :], T8[:, 2, :]) if hh == 0 else (rt[0], rt[1], rt[2])
                n0, n1, n2 = (f"hn{hh}a", f"hn{hh}b", f"hn{hh}c") if hh == 0 else ("rt0", "rt1", "rt2")
                S.act(lambda e, bo=bo, o_sb=o_sb: e.activation(out=o_sb, in_=P[:, bo, :], func=AF.Copy), writes=[f"ps{bo}", n0])
                S.act(lambda e, bo=bo, osq=osq: e.activation(out=osq, in_=P[:, bo, :], func=AF.Square), writes=[f"ps{bo}", n1])
                S.pe(lambda e, o_sb=o_sb: e.matmul(P[:, bm, :], lhsT=self.ones_f[:], rhs=o_sb, start=True, stop=True), reads=["c_ones", n0], writes=[f"ps{bm}"])
                S.pe(lambda e, osq=osq, bq=bq: e.matmul(P[:, bq, :], lhsT=self.ones_f[:], rhs=osq, start=True, stop=True), reads=["c_ones", n1], writes=[f"ps{bq}"])
                S.act(lambda e, osq=osq: e.activation(out=osq, in_=P[:, bm, :], func=AF.Copy), reads=[n1], writes=[f"ps{bm}", n1])
                S.act(lambda e, w2=w2: e.activation(out=w2, in_=P[:, bm, :], func=AF.Square), writes=[f"ps{bm}", n2])
                S.dve(lambda e, w2=w2, bq=bq: e.tensor_tensor(out=w2, in0=P[:, bq, :], in1=w2, op=ALU.subtract), reads=[n2], writes=[f"ps{bq}", n2])
                S.act(lambda e, w2=w2: e.activation(out=w2, in_=w2, func=AF.Sqrt, bias=HN_EPS, scale=1.0), reads=[n2], writes=[n2])
                S.dve(lambda e, w2=w2: e.reciprocal(out=w2, in_=w2), reads=[n2], writes=[n2])
                S.dve(lambda e, o_sb=o_sb, osq=osq: e.tensor_tensor(out=o_sb, in0=o_sb, in1=osq, op=ALU.subtract), reads=[n0, n1], writes=[n0])
                S.dve(lambda e, o_sb=o_sb, w2=w2: e.tensor_tensor(out=o_sb, in0=o_sb, in1=w2, op=ALU.mult), reads=[n0, n2], writes=[n0])
                S.dve(lambda e, o_sb=o_sb, hh=hh: e.tensor_tensor(out=rT4[:, :, hh, tsl], in0=o_sb.rearrange("p (c i) -> p c i", c=4), in1=rT4[:, :, hh, tsl], op=ALU.mult),
                      reads=[n0, f"rT{tl}"], writes=[f"rT{tl}"])

        stageA(0)
        stageBf(0)
        for tl in range(8):
            if tl + 1 < 8:
                stageA(tl + 1)
                stageBf(tl + 1)
            stageBb(tl)
        self.dump("onT", rT, [128, 8, 1024], [f"rT{tl}" for tl in range(8)])
        if DEBUG["stop"] == "retr":
            return
        self.dump("rT", rT, [128, 8, 1024], [f"rT{tl}" for tl in range(8)])


    def phase_tail(self, l, half):
        S, dr, P = self.S, self.dr, self.P
        V = self.big_views()
        rT, mergedT, rt = V["rT"], V["mergedT"], V["rt"]
        uz = self.uz
        tok0 = half * 1024
        self.load_ln_params(1 + l)
        if half == 0:
            S.dma("sp", lambda e: e.dma_start(out=self.wr_f[:], in_=dr["w_router"][l].rearrange("(c p) n -> p c n", p=128)), writes=["wr_f"])
            S.dma("sp", lambda e: e.dma_start(out=self.rb[:], in_=dr["b_router"][l].partition_broadcast(128)), writes=["rb"])
        step = 0
        for bb in range(4):
            s_gr = self.load_w(dr["w_in"][l, :, GROFF + 256 * bb:GROFF + 256 * (bb + 1)], 8, 256)
            s_gs = self.load_w(dr["w_in"][l, :, GSOFF + 256 * bb:GSOFF + 256 * (bb + 1)], 8, 256)
            s_br = self.load_w(dr["w_branch_ret"][l, :, 256 * bb:256 * (bb + 1)], 8, 256)
            s_bs = self.load_w(dr["w_branch_s5"][l, :, 256 * bb:256 * (bb + 1)], 4, 256)
            for tt in range(2):
                g0 = tok0 + tt * 512
                lsl = slice(tt * 512, (tt + 1) * 512)
                hts = [f"hT{8 * half + 4 * tt + k}" for k in range(4)]
                for o2 in range(2):
                    dc = 2 * bb + o2
                    base = 4 * (step % 2)
                    ta, tb = rt[2 * (step % 2)], rt[2 * (step % 2) + 1]
                    na, nb_ = f"rt{2 * (step % 2)}", f"rt{2 * (step % 2) + 1}"
                    step += 1
                    csl = slice(o2 * 128, (o2 + 1) * 128)
                    for c in range(8):
                        S.pe(lambda e, c=c, base=base, csl=csl, g0=g0, s_gr=s_gr: e.matmul(P[:, base, :], lhsT=self.wsl[:, s_gr, c, csl], rhs=self.hT[:, c, g0:g0 + 512], start=(c == 0), stop=(c == 7)),
                             reads=[f"w{s_gr}"] + hts, writes=[f"ps{base}"])
                    for c in range(8):
                        S.pe(lambda e, c=c, base=base, csl=csl, g0=g0, s_gs=s_gs: e.matmul(P[:, base + 1, :], lhsT=self.wsl[:, s_gs, c, csl], rhs=self.hT[:, c, g0:g0 + 512], start=(c == 0), stop=(c == 7)),
                             reads=[f"w{s_gs}"] + hts, writes=[f"ps{base + 1}"])
                    for c in range(8):
                        S.pe(lambda e, c=c, base=base, csl=csl, lsl=lsl, s_br=s_br: e.matmul(P[:, base + 2, :], lhsT=self.wsl[:, s_br, c, csl], rhs=rT[:, c, lsl], start=(c == 0), stop=(c == 7)),
                             reads=[f"w{s_br}"] + [f"rT{4 * tt + k}" for k in range(4)], writes=[f"ps{base + 2}"])
                    for c in range(4):
                        S.pe(lambda e, c=c, base=base, csl=csl, g0=g0, s_bs=s_bs: e.matmul(P[:, base + 3, :], lhsT=self.wsl[:, s_bs, c, csl], rhs=uz[:, c, g0:g0 + 512], start=(c == 0), stop=(c == 3)),
                             reads=[f"w{s_bs}"] + [f"uz{c}_{(g0 // 512)}"], writes=[f"ps{base + 3}"])
                    S.act(lambda e, base=base, ta=ta: e.activation(out=ta, in_=P[:, base, :], func=AF.Sigmoid), writes=[f"ps{base}", na])
                    S.act(lambda e, base=base, tb=tb: e.activation(out=tb, in_=P[:, base + 1, :], func=AF.Sigmoid), writes=[f"ps{base + 1}", nb_])
                    S.dve(lambda e, base=base, ta=ta: e.tensor_tensor(out=ta, in0=P[:, base + 2, :], in1=ta, op=ALU.mult), reads=[na], writes=[f"ps{base + 2}", na])
                    S.dve(lambda e, base=base, tb=tb: e.tensor_tensor(out=tb, in0=P[:, base + 3, :], in1=tb, op=ALU.mult), reads=[nb_], writes=[f"ps{base + 3}", nb_])
                    S.dve(lambda e, ta=ta, tb=tb, dc=dc, lsl=lsl: e.tensor_tensor(out=mergedT[:, dc, lsl], in0=ta, in1=tb, op=ALU.add), reads=[na, nb_], writes=[f"mg{dc}_{tt}"])
        self.dump("mergedT", mergedT, [128, 8, 1024], [f"mg{dc}_{tt}" for dc in range(8) for tt in range(2)])
        if DEBUG["stop"] == "tailm":
            return
        for ob in range(2):
            self.load_wbig(dr["w_out"][l, :, 512 * ob:512 * (ob + 1)], 8, ob)
        xv = self.T8[:].rearrange("p a f -> p (a f)").rearrange("p (b d) -> p b d", b=2)
        for tl in range(8):
            T = 8 * half + tl
            pb = tl % 2
            xb = xv[:, pb, :]
            R_x = f"xb{pb}"
            S.dma("sp", lambda e, T=T, xb=xb: e.dma_start(out=xb, in_=self.spillA[T * 128:(T + 1) * 128, :]), writes=[R_x], key=f"rl{pb}")
            for ob in range(2):
                for c in range(8):
                    S.pe(lambda e, c=c, ob=ob, tl=tl: e.matmul(P[:, 6 + ob, :], lhsT=mergedT[:, c, tl * 128:(tl + 1) * 128], rhs=self.wbig(ob)[:, c, :], start=(c == 0), stop=(c == 7)),
                         reads=[f"w{2 * ob}", f"w{2 * ob + 1}"] + [f"mg{c}_{tl // 4}"], writes=[f"ps{6 + ob}"])
                S.dve(lambda e, ob=ob, xb=xb: e.scalar_tensor_tensor(out=xb[:, ob * 512:(ob + 1) * 512], in0=xb[:, ob * 512:(ob + 1) * 512], scalar=ALPHA, in1=P[:, 6 + ob, :], op0=ALU.mult, op1=ALU.add),
                      reads=[R_x], writes=[R_x, f"ps{6 + ob}"])
            self.ln_tile(xb, R_x, T, pb, router=True, make_hT=True)
            S.act(lambda e, xb=xb: e.activation(out=xb, in_=xb, func=AF.Identity, scale=ALPHA), reads=[R_x], writes=[R_x])
            S.dma("sp", lambda e, T=T, xb=xb: e.dma_start(out=self.spillB[T * 128:(T + 1) * 128, :], in_=xb), reads=[R_x], key=f"sb{pb}")

    def phase_moe(self, l, last):
        S, dr, P = self.S, self.dr, self.P
        h_tm = self.h_tm
        for T in range(NT):
            S.dma("sp", lambda e, T=T: e.dma_start(out=h_tm[:, T, :], in_=self.spillB[T * 128:(T + 1) * 128, :]), writes=[f"htm{T}"], key=f"ld{T % 4}")
        self.load_ln_params(1 + DEPTH + l)
        self.dump(f"hmix{l}_tm", h_tm, [128, NT, D], [f"htm{T}" for T in range(NT)])
        rtmp = self.uz[:].rearrange("p c t -> p (c t)").bitcast(F32)
        off = [0]
        def tmp(k, shape):
            n = int(np.prod(shape))
            ap = rtmp[:, off[0]:off[0] + n]
            off[0] += n
            if len(shape) == 2:
                return ap.rearrange("p (t a) -> p t a", t=NT)
            if len(shape) == 3:
                return ap.rearrange("p (t a b) -> p t a b", t=NT, a=shape[1])
            return ap
        Lg = self.logit
        G = Lg[:, :, 0:4]
        E4 = Lg[:, :, 4:36].rearrange("p t (g e) -> p t g e", g=4)
        lrs = [f"logit{T}" for T in range(NT)]
        gmax = tmp(0, [NT]); gsh = tmp(1, [NT, 4]); gsum = tmp(2, [NT]); oh = tmp(3, [NT, 4]); t4 = tmp(4, [NT, 4, 8])
        esel = tmp(5, [NT, 8]); m1 = tmp(6, [NT]); mk1 = tmp(7, [NT, 8]); e2 = tmp(8, [NT, 8]); m2 = tmp(9, [NT]); mk2 = tmp(10, [NT, 8])
        p2 = tmp(11, [NT]); w1 = tmp(12, [NT]); w2 = tmp(13, [NT]); wl = tmp(14, [NT, 8]); wl2 = tmp(15, [NT, 8])
        bc = lambda ap, n: ap.unsqueeze(2).broadcast_to([128, NT, n])
        S.dve(lambda e: e.tensor_reduce(out=gmax, in_=G, axis=AX.X, op=ALU.max), reads=lrs, writes=["r_gmax"])
        S.dve(lambda e: e.tensor_tensor(out=gsh, in0=G, in1=bc(gmax, 4), op=ALU.subtract), reads=lrs + ["r_gmax"], writes=["r_gsh"])
        S.dve(lambda e: e.tensor_tensor(out=oh, in0=G, in1=bc(gmax, 4), op=ALU.is_equal), reads=lrs + ["r_gmax"], writes=["r_oh"])
        S.act(lambda e: e.activation(out=gsh, in_=gsh, func=AF.Exp), reads=["r_gsh"], writes=["r_gsh"])
        S.dve(lambda e: e.tensor_reduce(out=gsum, in_=gsh, axis=AX.X, op=ALU.add), reads=["r_gsh"], writes=["r_gsum"])
        S.dve(lambda e: e.reciprocal(out=gsum, in_=gsum), reads=["r_gsum"], writes=["r_gsum"])
        S.dve(lambda e: e.tensor_tensor(out=t4, in0=E4, in1=oh.unsqueeze(3).broadcast_to([128, NT, 4, 8]), op=ALU.mult), reads=lrs + ["r_oh"], writes=["r_t4"])
        S.dve(lambda e: e.tensor_tensor(out=esel, in0=t4[:, :, 0, :], in1=t4[:, :, 1, :], op=ALU.add), reads=["r_t4"], writes=["r_esel"])
        S.dve(lambda e: e.tensor_tensor(out=esel, in0=esel, in1=t4[:, :, 2, :], op=ALU.add), reads=["r_t4", "r_esel"], writes=["r_esel"])
        S.dve(lambda e: e.tensor_tensor(out=esel, in0=esel, in1=t4[:, :, 3, :], op=ALU.add), reads=["r_t4", "r_esel"], writes=["r_esel"])
        S.dve(lambda e: e.tensor_reduce(out=m1, in_=esel, axis=AX.X, op=ALU.max), reads=["r_esel"], writes=["r_m1"])
        S.dve(lambda e: e.tensor_tensor(out=mk1, in0=esel, in1=bc(m1, 8), op=ALU.is_equal), reads=["r_esel", "r_m1"], writes=["r_mk1"])
        S.dve(lambda e: e.scalar_tensor_tensor(out=e2, in0=mk1, scalar=-1e30, in1=esel, op0=ALU.mult, op1=ALU.add), reads=["r_mk1", "r_esel"], writes=["r_e2"])
        S.dve(lambda e: e.tensor_reduce(out=m2, in_=e2, axis=AX.X, op=ALU.max), reads=["r_e2"], writes=["r_m2"])
        S.dve(lambda e: e.tensor_tensor(out=mk2, in0=e2, in1=bc(m2, 8), op=ALU.is_equal), reads=["r_e2", "r_m2"], writes=["r_mk2"])
        S.dve(lambda e: e.tensor_tensor(out=p2, in0=m2, in1=m1, op=ALU.subtract), reads=["r_m1", "r_m2"], writes=["r_p2"])
        S.act(lambda e: e.activation(out=p2, in_=p2, func=AF.Exp), reads=["r_p2"], writes=["r_p2"])
        S.dve(lambda e: e.tensor_scalar(out=w1, in0=p2, scalar1=1.0, scalar2=None, op0=ALU.add), reads=["r_p2"], writes=["r_w1"])
        S.dve(lambda e: e.reciprocal(out=w1, in_=w1), reads=["r_w1"], writes=["r_w1"])
        S.dve(lambda e: e.tensor_tensor(out=w2, in0=p2, in1=w1, op=ALU.mult), reads=["r_p2", "r_w1"], writes=["r_w2"])
        S.dve(lambda e: e.tensor_tensor(out=w1, in0=w1, in1=gsum, op=ALU.mult), reads=["r_w1", "r_gsum"], writes=["r_w1"])
        S.dve(lambda e: e.tensor_tensor(out=w2, in0=w2, in1=gsum, op=ALU.mult), reads=["r_w2", "r_gsum"], writes=["r_w2"])
        S.dve(lambda e: e.tensor_tensor(out=wl, in0=mk1, in1=bc(w1, 8), op=ALU.mult), reads=["r_mk1", "r_w1"], writes=["r_wl"])
        S.dve(lambda e: e.tensor_tensor(out=wl2, in0=mk2, in1=bc(w2, 8), op=ALU.mult), reads=["r_mk2", "r_w2"], writes=["r_wl2"])
        S.dve(lambda e: e.tensor_tensor(out=wl, in0=wl, in1=wl2, op=ALU.add), reads=["r_wl", "r_wl2"], writes=["r_wl"])
        comb4 = self.comb[:].rearrange("p t (g e) -> p t g e", g=4)
        S.dve(lambda e: e.tensor_tensor(out=comb4, in0=oh.unsqueeze(3).broadcast_to([128, NT, 4, 8]), in1=wl.unsqueeze(2).broadcast_to([128, NT, 4, 8]), op=ALU.mult),
              reads=["r_oh", "r_wl"], writes=["comb"])
        self.dump(f"comb{l}", self.comb[:], [128, NT, 32], ["comb"])
        wd = self.Y16[:, 0:6144].rearrange("p (s c n) -> p s c n", s=3, c=2)
        abuf = self.PH[:, 2048:4096].bitcast(BF16).rearrange("p (s k f n) -> p s k f n", s=2, k=2, f=2)
        steps = [(e_, tt) for e_ in range(NEXP) for tt in range(4)]
        info = {}

        def gateup(si):
            e_, tt = steps[si]
            if tt == 0:
                sg_ = self.load_w(dr["w_exp_gate"][l, e_], 8, 256)
                su_ = self.load_w(dr["w_exp_up"][l, e_], 8, 256)
                ws = e_ % 3
                S.dma("pool", lambda e, ws=ws, e_=e_: e.dma_start(out=wd[:, ws, :, :], in_=dr["w_exp_down"][l, e_].rearrange("(c p) n -> p c n", p=128)), writes=[f"wd{ws}"], key=f"wd{ws}")
                info[e_] = (sg_, su_, ws)
            sg_, su_, ws = info[e_]
            st_ = si % 2
            hts = [f"hT{4 * tt + k}" for k in range(4)]
            for fc in range(2):
                for kind, slot in ((0, sg_), (1, su_)):
                    bk = 2 * kind + fc
                    for c in range(8):
                        S.pe(lambda e, c=c, bk=bk, slot=slot, fc=fc, tt=tt: e.matmul(P[:, bk, :], lhsT=self.wsl[:, slot, c, fc * 128:(fc + 1) * 128], rhs=self.hT[:, c, tt * 512:(tt + 1) * 512], start=(c == 0), stop=(c == 7)),
                             reads=[f"w{slot}"] + hts, writes=[f"ps{bk}"])
            for fc in range(2):
                S.act(lambda e, fc=fc, st_=st_: e.activation(out=abuf[:, st_, 0, fc, :], in_=P[:, fc, :], func=AF.Silu), writes=[f"ps{fc}", f"ab{st_}sg{fc}"])
                S.dve(lambda e, fc=fc, st_=st_: e.tensor_tensor(out=abuf[:, st_, 1, fc, :], in0=P[:, 2 + fc, :], in1=abuf[:, st_, 0, fc, :], op=ALU.mult),
                      reads=[f"ab{st_}sg{fc}"], writes=[f"ps{2 + fc}", f"ab{st_}a{fc}"])

        dn = [0]

        def down(si):
            e_, tt = steps[si]
            sg_, su_, ws = info[e_]
            st_ = si % 2
            for ts in range(4):
                T = 4 * tt + ts
                for dh in range(2):
                    bk = 4 + dn[0] % 4
                    dn[0] += 1
                    for fc in range(2):
                        S.pe(lambda e, fc=fc, bk=bk, ts=ts, dh=dh, st_=st_, ws=ws: e.matmul(P[:, bk, :], lhsT=abuf[:, st_, 1, fc, ts * 128:(ts + 1) * 128], rhs=wd[:, ws, fc, dh * 512:(dh + 1) * 512], start=(fc == 0), stop=(fc == 1)),
                             reads=[f"ab{st_}a{fc}", f"wd{ws}"], writes=[f"ps{bk}"])
                    S.dve(lambda e, bk=bk, T=T, dh=dh, e_=e_: e.scalar_tensor_tensor(out=h_tm[:, T, dh * 512:(dh + 1) * 512], in0=P[:, bk, :], scalar=self.comb[:, T, e_:e_ + 1], in1=h_tm[:, T, dh * 512:(dh + 1) * 512], op0=ALU.mult, op1=ALU.add),
                          reads=["comb", f"htm{T}"], writes=[f"htm{T}", f"ps{bk}"])

        nexp_run = int(os.environ.get("NEXP_RUN", NEXP))
        nsteps = nexp_run * 4
        for si in range(nsteps + 1):
            if si < nsteps:
                gateup(si)
            if si >= 1:
                down(si - 1)
        for T in range(NT):
            self.ln_tile(h_tm[:, T, :], f"htm{T}", T, T % 2, router=False, make_hT=not last)
            if last:
                S.dma("sp", lambda e, T=T: e.dma_start(out=self.out[T * 128:(T + 1) * 128, :], in_=h_tm[:, T, :]), reads=[f"htm{T}"], key=f"out{T % 4}")
            else:
                S.dma("sp", lambda e, T=T: e.dma_start(out=self.spillA[T * 128:(T + 1) * 128, :], in_=h_tm[:, T, :]), reads=[f"htm{T}"], key=f"sp{T % 4}")
        self.dump(f"hffn{l}_tm", h_tm, [128, NT, D], [f"htm{T}" for T in range(NT)])


def build(nc, st):
    B = Builder(nc, st)
    B.consts()
    B.phase_in()
    B.S.barrier()
    if DEBUG["stop"] == "in":
        return B
    for l in range(DEPTH):
        B.phase_s5(l)
        B.S.barrier()
        if DEBUG["stop"] == "s5":
            return B
        for half in range(2):
            B.phase_ret(l, half)
            B.S.barrier()
            if DEBUG["stop"] in ("ret", "retqk", "retv", "retr"):
                return B
            B.phase_tail(l, half)
            B.S.barrier()
            if DEBUG["stop"] in ("tail", "tailm"):
                return B
        B.phase_moe(l, last=(l == DEPTH - 1))
        B.S.barrier()
        if DEBUG["stop"] == f"moe{l}":
            return B
    return B


def kernel(**inputs):
    x = np.ascontiguousarray(np.asarray(inputs["x"], dtype=np.float32))
    params = host_layout(inputs)
    params.update(host_constants())
    nc = bass.Bass("TRN2", target_bir_lowering=False)
    with ExitStack() as st:
        B = build(nc, st)
        B.S.emit(st)
    in_maps = []
    for b in range(8):
        m = {"x": x[b]}
        m.update(params)
        in_maps.append(m)
    res = run_bass_kernel_spmd(nc, in_maps, core_ids=list(range(8)))
    return np.stack([np.asarray(r["out"], dtype=np.float32) for r in res.results], axis=0)
```

```python
import math
import os
from contextlib import ExitStack
import numpy as np
import concourse.bass as bass
import concourse.mybir as mybir
from concourse.bass_utils import run_bass_kernel_spmd

F32 = mybir.dt.float32
BF16 = mybir.dt.bfloat16
I32 = mybir.dt.int32
AF = mybir.ActivationFunctionType
ALU = mybir.AluOpType
AX = mybir.AxisListType

D = 1024
SEQ = 2048
DEPTH = 2
NT = 16
INW = 5632
QOFF, KOFF, VOFF, GOFF, UOFF, GROFF, GSOFF = 0, 512, 1024, 2048, 3072, 3584, 4608
NEXP = 32
ALPHA = (2 * DEPTH) ** 0.25
LN_EPS = 1e-5
HN_EPS = 1e-6
TWO_PI = 2.0 * math.pi
POWS = [1, 2, 3, 4, 5, 6, 7, 8, 16, 32, 64, 128, 256, 512, 1024]

DEBUG = {"dumps": None, "stop": None}


class Sched:
    ENGS = ("pe", "act", "dve", "pool", "sp")

    def __init__(self, nc):
        self.nc = nc
        self.ops = []

    def add(self, eng, fn, reads=(), writes=(), dma=None, nobar=False):
        self.ops.append(dict(eng=eng, fn=fn, reads=tuple(reads), writes=tuple(writes), dma=dma, bar=False, nobar=nobar))

    def pe(self, fn, reads=(), writes=()):
        self.add("pe", fn, reads, writes)

    def act(self, fn, reads=(), writes=()):
        self.add("act", fn, reads, writes)

    def dve(self, fn, reads=(), writes=()):
        self.add("dve", fn, reads, writes)

    def pool(self, fn, reads=(), writes=()):
        self.add("pool", fn, reads, writes)

    def dma(self, eng, fn, reads=(), writes=(), key=None, nobar=False):
        if key is None:
            key = writes[0]
        self.add(eng, fn, reads, writes, dma=key, nobar=nobar)

    def barrier(self):
        self.ops.append(dict(eng=None, fn=None, reads=(), writes=(), dma=None, bar=True, nobar=False))

    def emit(self, stack):
        nc = self.nc
        ops = self.ops
        n = len(ops)
        last_w, readers = {}, {}
        deps = [()] * n
        signals = [False] * n
        last_on = {e: None for e in self.ENGS}
        bar_deps = []
        seen_dma = []
        for i, op in enumerate(ops):
            if op["bar"]:
                bar_deps = [j for j in last_on.values() if j is not None] + list(seen_dma)
                seen_dma = []
                continue
            d = set() if op["nobar"] else set(bar_deps)
            raw = set()
            for r in op["reads"]:
                if r in last_w:
                    d.add(last_w[r]); raw.add(last_w[r])
            for w in op["writes"]:
                if w in last_w:
                    d.add(last_w[w])
                d.update(readers.get(w, ()))
            need = []
            for j in d:
                oj = ops[j]
                if oj["dma"] is not None:
                    need.append(j)
                elif oj["eng"] == op["eng"]:
                    if op["dma"] is not None or op["eng"] != "pe":
                        need.append(j)
                else:
                    need.append(j)
            for j in need:
                if ops[j]["dma"] is None:
                    signals[j] = True
            deps[i] = need
            for r in op["reads"]:
                readers.setdefault(r, []).append(i)
            for w in op["writes"]:
                last_w[w] = i
                readers[w] = []
            if not op["nobar"]:
                last_on[op["eng"]] = i
            if op["dma"] is not None:
                seen_dma.append(i)
        esem = {e: stack.enter_context(nc.semaphore(f"s_{e}")) for e in self.ENGS}
        dkeys = []
        for op in ops:
            if op["dma"] is not None and op["dma"] not in dkeys:
                dkeys.append(op["dma"])
        dsem = {k: stack.enter_context(nc.semaphore(f"d_{idx}")) for idx, k in enumerate(dkeys)}
        cnt = {e: 0 for e in self.ENGS}
        dcnt = {k: 0 for k in dkeys}
        sigval = [0] * n
        for i, op in enumerate(ops):
            if op["bar"]:
                continue
            if op["dma"] is not None:
                dcnt[op["dma"]] += 16
                sigval[i] = dcnt[op["dma"]]
            elif signals[i]:
                cnt[op["eng"]] += 1
                sigval[i] = cnt[op["eng"]]
        waited = {e: {} for e in self.ENGS}
        plan = {e: [] for e in self.ENGS}
        dtot = {k: 0 for k in dkeys}
        nwaits = 0
        for i, op in enumerate(ops):
            if op["bar"]:
                continue
            if op["dma"] is not None:
                dtot[op["dma"]] += 16
            e = op["eng"]
            ws = {}
            for j in deps[i]:
                oj = ops[j]
                if oj["dma"] is not None:
                    key = ("d", oj["dma"])
                    val = dtot[oj["dma"]] if op["dma"] != oj["dma"] else sigval[j]
                else:
                    key = ("e", oj["eng"])
                    val = sigval[j]
                if waited[e].get(key, 0) >= val:
                    continue
                ws[key] = max(ws.get(key, 0), val)
            for key, val in ws.items():
                waited[e][key] = val
            nwaits += len(ws)
            plan[e].append((i, list(ws.items())))
        self.stats = dict(n_ops={e: len(plan[e]) for e in self.ENGS}, signals=dict(cnt), dma_keys=len(dkeys), waits=nwaits)
        block = stack.enter_context(nc.Block())

        def run(engname):
            def body(eng):
                for i, ws in plan[engname]:
                    op = ops[i]
                    for key, val in ws:
                        eng.wait_ge(dsem[key[1]] if key[0] == "d" else esem[key[1]], val)
                    inst = op["fn"](eng)
                    if op["dma"] is not None:
                        inst.then_inc(dsem[op["dma"]], 16)
                    elif signals[i]:
                        inst.then_inc(esem[engname], 1)
                for k in dkeys:
                    if any((not o["bar"]) and o["dma"] == k and o["eng"] == engname for o in ops):
                        eng.wait_ge(dsem[k], dcnt[k])
            return body

        block.tensor(run("pe"))
        block.scalar(run("act"))
        block.vector(run("dve"))
        block.gpsimd(run("pool"))
        block.sync(run("sp"))


def gammas():
    return [1.0 - 2.0 ** (-5.0 - h) for h in range(8)]


def host_constants():
    c = {}
    c["c_ident"] = np.eye(128, dtype=np.float32)
    half = 32
    inv_freq = (10000.0 ** (-np.arange(half, dtype=np.float32) / half)).astype(np.float32)
    ang = np.arange(SEQ, dtype=np.float32)[:, None] * inv_freq[None, :]
    cos, sin = np.cos(ang).astype(np.float32), np.sin(ang).astype(np.float32)
    rope = np.zeros((2, 128, SEQ), np.float32)
    for p in range(128):
        d = p % 64
        f = d % 32
        rope[0, p] = cos[:, f]
        rope[1, p] = -sin[:, f] if d < 32 else sin[:, f]
    c["c_rope"] = rope
    g = gammas()
    intra = np.zeros((128, 8, 128), np.float32)
    jj = np.arange(128)
    for h in range(8):
        m = (g[h] ** np.abs(jj[:, None] - jj[None, :])) * 0.125
        m = m * ((jj[:, None] // 64) == (jj[None, :] // 64))
        intra[:, h, :] = m
    c["c_intra"] = intra
    kdec = np.zeros((128, 2, 8), np.float32)
    for h in range(8):
        for j in range(64):
            kdec[j, 0, h] = (g[h] ** (63 - j)) * 0.125
            kdec[64 + j, 1, h] = (g[h] ** (63 - j)) * 0.125
    c["c_kdec"] = kdec
    qdec = np.zeros((128, 4, 64), np.float32)
    for p in range(128):
        for c4 in range(4):
            h = 2 * c4 + p // 64
            qdec[p, c4, :] = g[h] ** (np.arange(64) + 1.0)
    c["c_qdec"] = qdec
    sdec = np.zeros((128, 8), np.float32)
    for h in range(8):
        sdec[:, h] = g[h] ** 64
    c["c_sdec"] = sdec
    c["c_ones"] = np.full((128, 128), 1.0 / 128.0, np.float32)
    return c


def host_layout(inp):
    L = DEPTH
    o = {}
    f = lambda a: np.ascontiguousarray(np.asarray(a, dtype=np.float32))
    w_in = f(inp["w_in"])
    o["w_in"] = w_in
    perm = np.zeros(512, np.int64)
    for h in range(8):
        for d in range(64):
            perm[h * 64 + d] = h * 64 + (d + 32 if d < 32 else d - 32)
    o["w_qksw"] = np.ascontiguousarray(np.concatenate([w_in[:, :, QOFF + perm], w_in[:, :, KOFF + perm]], axis=2))
    for k in ("w_glu", "w_branch_ret", "w_branch_s5", "w_out", "w_exp_gate", "w_exp_up", "w_exp_down"):
        o[k] = f(inp[k])
    o["w_router"] = np.ascontiguousarray(np.concatenate([f(inp["w_router_group"]), f(inp["w_router_expert"])], axis=2))
    o["b_router"] = np.ascontiguousarray(np.concatenate([f(inp["b_router_group"]), f(inp["b_router_expert"])], axis=1))
    lng = np.stack([f(inp["ln_in_g"])] + [f(inp["ln_mix_g"])[l] for l in range(L)] + [f(inp["ln_ffn_g"])[l] for l in range(L)])
    lnb = np.stack([f(inp["ln_in_b"])] + [f(inp["ln_mix_b"])[l] for l in range(L)] + [f(inp["ln_ffn_b"])[l] for l in range(L)])
    o["ln_gb"] = np.ascontiguousarray(np.stack([lng, lnb], axis=1))
    bre, bim = f(inp["s5_b_re"]), f(inp["s5_b_im"])
    cre, cim = f(inp["s5_c_re"]), f(inp["s5_c_im"])
    BB = np.zeros((L, 2, 128, 16, 128), np.float32)
    CC = np.zeros((L, 2, 128, 16, 128), np.float32)
    for g in range(32):
        j, g2, gl = g // 2, g % 2, g % 8
        BB[:, 0, gl * 16:(gl + 1) * 16, j, g2 * 64:(g2 + 1) * 64] = np.transpose(bre[:, g], (0, 2, 1))
        BB[:, 1, gl * 16:(gl + 1) * 16, j, g2 * 64:(g2 + 1) * 64] = np.transpose(bim[:, g], (0, 2, 1))
        CC[:, 0, g2 * 64:(g2 + 1) * 64, j, gl * 16:(gl + 1) * 16] = np.transpose(cre[:, g], (0, 2, 1))
        CC[:, 1, g2 * 64:(g2 + 1) * 64, j, gl * 16:(gl + 1) * 16] = np.transpose(cim[:, g], (0, 2, 1))
    o["s5_BB"], o["s5_CC"] = BB, CC
    lre, lim, lst = f(inp["s5_lambda_re"]), f(inp["s5_lambda_im"]), f(inp["s5_log_step"])
    lste = np.repeat(lst[:, :, None], 64, axis=2)
    o["s5_lamb"] = np.ascontiguousarray(np.stack([lre, lim, lste], axis=1).reshape(L, 3, 2048))
    lamp = np.zeros((L, 128, 3, 16), np.float32)
    for g in range(32):
        j, g2 = g // 2, g % 2
        lamp[:, g2 * 64:(g2 + 1) * 64, 0, j] = lre[:, g]
        lamp[:, g2 * 64:(g2 + 1) * 64, 1, j] = lim[:, g]
        lamp[:, g2 * 64:(g2 + 1) * 64, 2, j] = lste[:, g]
    o["s5_lamp"] = lamp
    o["s5_dcol"] = np.ascontiguousarray(np.transpose(f(inp["s5_d"]).reshape(L, 4, 128), (0, 2, 1)))
    return o


PARAM_SHAPES = {
    "w_in": [DEPTH, D, INW], "w_qksw": [DEPTH, D, 1024], "w_glu": [DEPTH, 512, 512],
    "w_branch_ret": [DEPTH, D, D], "w_branch_s5": [DEPTH, 512, D], "w_out": [DEPTH, D, D],
    "w_exp_gate": [DEPTH, NEXP, D, 256], "w_exp_up": [DEPTH, NEXP, D, 256], "w_exp_down": [DEPTH, NEXP, 256, D],
    "w_router": [DEPTH, D, 36], "b_router": [DEPTH, 36], "ln_gb": [1 + 2 * DEPTH, 2, D],
    "s5_BB": [DEPTH, 2, 128, 16, 128], "s5_CC": [DEPTH, 2, 128, 16, 128], "s5_lamb": [DEPTH, 3, 2048],
    "s5_lamp": [DEPTH, 128, 3, 16], "s5_dcol": [DEPTH, 128, 4],
    "c_ident": [128, 128], "c_rope": [2, 128, SEQ], "c_intra": [128, 8, 128], "c_kdec": [128, 2, 8],
    "c_qdec": [128, 4, 64], "c_sdec": [128, 8], "c_ones": [128, 128],
}


class Builder:
    def __init__(self, nc, st):
        self.nc, self.st = nc, st
        self.S = Sched(nc)
        self.dumps = {}
        nc_ = nc
        self.x = nc.dram_tensor("x", [SEQ, D], F32, kind="ExternalInput").ap()
        self.out = nc.dram_tensor("out", [SEQ, D], F32, kind="ExternalOutput").ap()
        self.dr = {k: nc.dram_tensor(k, shp, F32, kind="ExternalInput").ap() for k, shp in PARAM_SHAPES.items()}
        self.spillA = nc.dram_tensor("spillA", [SEQ, D], F32, kind="Internal").ap()
        self.spillB = nc.dram_tensor("spillB", [SEQ, D], F32, kind="Internal").ap()
        sb = lambda name, shape, dt: st.enter_context(nc.sbuf_tensor(name, shape, dt))
        self.hT = sb("hT", [128, 8, SEQ], BF16)
        self.BIG = sb("BIG", [128, 16384], F32)
        self.uz = sb("uz", [128, 4, SEQ], BF16)
        self.Y16 = sb("Y16", [128, 8192], BF16)
        self.wsl = sb("wsl", [128, 6, 8, 256], BF16)
        self.PH = sb("PH", [128, 4096], F32)
        self.T8 = sb("T8", [128, 4, 512], F32)
        self.ident_f = sb("ident_f", [128, 128], F32)
        self.ident_b = sb("ident_b", [128, 128], BF16)
        self.ones_f = sb("ones_f", [128, 128], F32)
        self.intra = sb("intra", [128, 8, 128], F32)
        self.kdec = sb("kdec", [128, 2, 8], F32)
        self.qdec = sb("qdec", [128, 4, 64], F32)
        self.sdec = sb("sdec", [128, 8], F32)
        self.S32 = sb("S32", [128, 1024], F32)
        self.Sbf = sb("Sbf", [128, 5, 1024], BF16)
        self.logit = sb("logit", [128, NT, 36], F32)
        self.comb = sb("comb", [128, NT, 32], F32)
        self.wr_f = sb("wr_f", [128, 8, 36], F32)
        self.rb = sb("rb", [128, 36], F32)
        self.small = sb("small", [128, 256], F32)
        self.P = st.enter_context(nc.psum_tensor("P", [128, 8, 512], F32))
        self.h_tm = self.BIG[:].rearrange("p (t d) -> p t d", t=NT)
        self.wslot_n = 0
        self.eps_ln = None

    def dump(self, name, ap, shape, reads):
        dd = DEBUG["dumps"]
        if not dd or name not in dd or name in self.dumps:
            return
        t = self.nc.dram_tensor("dbg_" + name, list(shape), ap.dtype, kind="ExternalOutput").ap()
        self.dumps[name] = t
        self.S.dma("sp", lambda e: e.dma_start(out=t, in_=ap), reads=reads, key="dbg_" + name)

    def ps(self, b):
        return self.P[:, b, :]

    def next_slot(self):
        s = self.wslot_n % 6
        self.wslot_n += 1
        return s

    def load_w(self, src2d, kc, ncols, slot=None, col0=0):
        if slot is None:
            slot = self.next_slot()
        dst = self.wsl[:, slot, 0:kc, col0:col0 + ncols]
        src = src2d.rearrange("(c p) n -> p c n", p=128)
        self.S.dma("pool", lambda e: e.dma_start(out=dst, in_=src), writes=[f"w{slot}"], key=f"w{slot}_{col0}", nobar=True)
        return slot

    def consts(self):
        S, dr = self.S, self.dr
        for nm, dst in (("c_ident", self.ident_f), ("c_ones", self.ones_f), ("c_intra", self.intra), ("c_kdec", self.kdec),
                        ("c_qdec", self.qdec), ("c_sdec", self.sdec)):
            S.dma("sp", lambda e, d=dst, s=dr[nm]: e.dma_start(out=d[:], in_=s), writes=[nm])
        S.dve(lambda e: e.tensor_copy(out=self.ident_b[:], in_=self.ident_f[:]), reads=["c_ident"], writes=["ident_b"])

    def ln_tile(self, x, R_x, T, pb, router, make_hT):
        S = self.S
        sm = self.small
        st_ap = sm[:, pb * 16:pb * 16 + 12]
        mv = sm[:, 32 + pb * 4:32 + pb * 4 + 2]
        rstd = sm[:, 40 + pb:41 + pb]
        gbb = self.PH[:, 0:2048].rearrange("p (a d) -> p a d", a=2)
        for hh in range(2):
            S.dve(lambda e, hh=hh: e.bn_stats(out=st_ap[:, hh * 6:(hh + 1) * 6], in_=x[:, hh * 512:(hh + 1) * 512]), reads=[R_x], writes=[f"lnst{pb}"])
        S.dve(lambda e: e.bn_aggr(out=mv, in_=st_ap.rearrange("p (a b) -> p a b", b=6)), reads=[f"lnst{pb}"], writes=[f"lnmv{pb}"])
        S.act(lambda e: e.activation(out=rstd, in_=mv[:, 1:2], func=AF.Sqrt, bias=LN_EPS, scale=1.0), reads=[f"lnmv{pb}"], writes=[f"lnrs{pb}"])
        S.dve(lambda e: e.reciprocal(out=rstd, in_=rstd), reads=[f"lnrs{pb}"], writes=[f"lnrs{pb}"])
        S.dve(lambda e: e.scalar_tensor_tensor(out=x, in0=x, scalar=mv[:, 0:1], in1=gbb[:, 0, :], op0=ALU.subtract, op1=ALU.mult),
              reads=[R_x, f"lnmv{pb}", "lngb"], writes=[R_x])
        S.dve(lambda e: e.scalar_tensor_tensor(out=x, in0=x, scalar=rstd, in1=gbb[:, 1, :], op0=ALU.mult, op1=ALU.add),
              reads=[R_x, f"lnrs{pb}", "lngb"], writes=[R_x])
        if not make_hT:
            return
        for c in range(8):
            b = 2 * pb + c // 4
            S.pe(lambda e, c=c, b=b: e.transpose(out=self.P[:, b, (c % 4) * 128:(c % 4 + 1) * 128], in_=x[:, c * 128:(c + 1) * 128], identity=self.ident_f[:]),
                 reads=[R_x, "c_ident"], writes=[f"ps{b}"])
        tsl = slice(T * 128, (T + 1) * 128)
        if not router:
            for k in range(2):
                b = 2 * pb + k
                S.act(lambda e, k=k, b=b: e.activation(out=self.hT[:, 4 * k:4 * k + 4, tsl], in_=self.P[:, b, :].rearrange("p (c t) -> p c t", c=4), func=AF.Copy),
                      reads=[], writes=[f"ps{b}", f"hT{T}"])
        else:
            hTf = self.PH[:, 2048 + pb * 1024:2048 + (pb + 1) * 1024].rearrange("p (c t) -> p c t", c=8)
            for k in range(2):
                b = 2 * pb + k
                S.act(lambda e, k=k, b=b: e.activation(out=hTf[:, 4 * k:4 * k + 4, :], in_=self.P[:, b, :].rearrange("p (c t) -> p c t", c=4), func=AF.Copy),
                      reads=[], writes=[f"ps{b}", f"hTf{pb}"])
            for k in range(2):
                b = 2 * pb + k
                S.act(lambda e, k=k, b=b: e.activation(out=self.hT[:, 4 * k:4 * k + 4, tsl], in_=self.P[:, b, :].rearrange("p (c t) -> p c t", c=4), func=AF.Copy),
                      reads=[], writes=[f"ps{b}", f"hT{T}"])
            bl = 4 + pb
            for c in range(8):
                S.pe(lambda e, c=c: e.matmul(self.P[:, bl, 0:36], lhsT=hTf[:, c, :], rhs=self.wr_f[:, c, :], start=(c == 0), stop=(c == 7)),
                     reads=[f"hTf{pb}", "wr_f"], writes=[f"ps{bl}"])
            S.dve(lambda e: e.tensor_tensor(out=self.logit[:, T, :], in0=self.P[:, bl, 0:36], in1=self.rb[:], op=ALU.add),
                  reads=["rb"], writes=[f"ps{bl}", f"logit{T}"])

    def load_ln_params(self, idx):
        gbb = self.PH[:, 0:2048].rearrange("p (a d) -> p a d", a=2)
        for a in range(2):
            self.S.dma("sp", lambda e, a=a: e.dma_start(out=gbb[:, a, :], in_=self.dr["ln_gb"][idx, a].partition_broadcast(128)),
                       writes=["lngb"], key=f"lngb{a}")

    def phase_in(self):
        S = self.S
        self.load_ln_params(0)
        for T in range(NT):
            S.dma("sp", lambda e, T=T: e.dma_start(out=self.h_tm[:, T, :], in_=self.x[T * 128:(T + 1) * 128, :]), writes=[f"htm{T}"], key=f"ldx{T}")
        for T in range(NT):
            self.ln_tile(self.h_tm[:, T, :], f"htm{T}", T, T % 2, router=False, make_hT=True)
            S.dma("sp", lambda e, T=T: e.dma_start(out=self.spillA[T * 128:(T + 1) * 128, :], in_=self.h_tm[:, T, :]), reads=[f"htm{T}"], key=f"sp{T % 4}")
        self.dump("h0_tm", self.h_tm, [128, NT, D], [f"htm{T}" for T in range(NT)])
        self.dump("h0_T", self.hT[:], [128, 8, SEQ], [f"hT{T}" for T in range(NT)])


    def abar_tables(self, lre, lim, lst, tmp, tag):
        S = self.S
        R = lambda k: f"{tag}_t{k}"
        t = tmp
        S.act(lambda e: e.activation(out=lst, in_=lst, func=AF.Exp), reads=[f"{tag}_lst"], writes=[f"{tag}_lst"])
        S.dve(lambda e: e.tensor_scalar(out=lre, in0=lre, scalar1=-1e-4, scalar2=None, op0=ALU.min), reads=[f"{tag}_lre"], writes=[f"{tag}_lre"])
        S.dve(lambda e: e.tensor_tensor(out=t[2], in0=lre, in1=lst, op=ALU.mult), reads=[f"{tag}_lre", f"{tag}_lst"], writes=[R(2)])
        S.act(lambda e: e.activation(out=t[2], in_=t[2], func=AF.Exp), reads=[R(2)], writes=[R(2)])
        S.dve(lambda e: e.tensor_tensor(out=t[3], in0=lim, in1=lst, op=ALU.mult), reads=[f"{tag}_lim", f"{tag}_lst"], writes=[R(3)])
        S.dve(lambda e: e.tensor_scalar(out=t[4], in0=t[3], scalar1=1.0 / TWO_PI, scalar2=None, op0=ALU.mult), reads=[R(3)], writes=[R(4)])
        t5i = t[5].bitcast(I32)
        S.dve(lambda e: e.tensor_copy(out=t5i, in_=t[4]), reads=[R(4)], writes=[R(5)])
        S.dve(lambda e: e.tensor_copy(out=t[4], in_=t5i), reads=[R(5)], writes=[R(4)])
        S.dve(lambda e: e.scalar_tensor_tensor(out=t[3], in0=t[4], scalar=-TWO_PI, in1=t[3], op0=ALU.mult, op1=ALU.add), reads=[R(4), R(3)], writes=[R(3)])
        def wrap(x, k):
            S.dve(lambda e: e.tensor_scalar(out=t[k], in0=x, scalar1=math.pi, scalar2=-TWO_PI, op0=ALU.is_gt, op1=ALU.mult), reads=[R(3), R(4)], writes=[R(k)])
            S.dve(lambda e: e.tensor_tensor(out=x, in0=x, in1=t[k], op=ALU.add), reads=[R(3), R(4), R(k)], writes=[R(3), R(4)])
            S.dve(lambda e: e.tensor_scalar(out=t[k], in0=x, scalar1=-math.pi, scalar2=TWO_PI, op0=ALU.is_lt, op1=ALU.mult), reads=[R(3), R(4)], writes=[R(k)])
            S.dve(lambda e: e.tensor_tensor(out=x, in0=x, in1=t[k], op=ALU.add), reads=[R(3), R(4), R(k)], writes=[R(3), R(4)])
        wrap(t[3], 5)
        S.dve(lambda e: e.tensor_scalar(out=t[4], in0=t[3], scalar1=math.pi / 2, scalar2=None, op0=ALU.add), reads=[R(3)], writes=[R(4)])
        wrap(t[4], 5)
        S.act(lambda e: e.activation(out=t[3], in_=t[3], func=AF.Sin), reads=[R(3)], writes=[R(3)])
        S.act(lambda e: e.activation(out=t[4], in_=t[4], func=AF.Sin), reads=[R(4)], writes=[R(4)])
        S.dve(lambda e: e.tensor_tensor(out=t[0], in0=t[2], in1=t[4], op=ALU.mult), reads=[R(2), R(4)], writes=[R(0)])
        S.dve(lambda e: e.tensor_tensor(out=t[1], in0=t[2], in1=t[3], op=ALU.mult), reads=[R(2), R(3)], writes=[R(1)])
        return t[0], t[1]

    def cmul(self, ore, oim, are, aim, bre, bim, t, tag):
        S = self.S
        S.dve(lambda e: e.tensor_tensor(out=t[0], in0=are, in1=bre, op=ALU.mult), reads=[tag], writes=[tag + "x0"])
        S.dve(lambda e: e.tensor_tensor(out=t[1], in0=aim, in1=bim, op=ALU.mult), reads=[tag], writes=[tag + "x1"])
        S.dve(lambda e: e.tensor_tensor(out=ore, in0=t[0], in1=t[1], op=ALU.subtract), reads=[tag + "x0", tag + "x1"], writes=[tag])
        S.dve(lambda e: e.tensor_tensor(out=t[0], in0=are, in1=bim, op=ALU.mult), reads=[tag], writes=[tag + "x0"])
        S.dve(lambda e: e.tensor_tensor(out=t[1], in0=aim, in1=bre, op=ALU.mult), reads=[tag], writes=[tag + "x1"])
        S.dve(lambda e: e.tensor_tensor(out=oim, in0=t[0], in1=t[1], op=ALU.add), reads=[tag + "x0", tag + "x1"], writes=[tag])

    def phase_s5(self, l):
        S, dr = self.S, self.dr
        BIG, PH, Y16, uz = self.BIG, self.PH, self.Y16, self.uz
        W32 = self.wsl[:].rearrange("p s c n -> p (s c n)").bitcast(F32)
        T8f = self.T8[:].rearrange("p a f -> p (a f)")
        SA = [[BIG[:, 0:2048], BIG[:, 2048:4096]], [BIG[:, 4096:6144], BIG[:, 6144:8192]]]
        SB = [[BIG[:, 4096:6144], BIG[:, 6144:8192]], [W32[:, 4096:6144], T8f]]
        hb = BIG[:, 8192:12288].bitcast(BF16).rearrange("p (b c t) -> p b c t", b=2, c=2)
        tp = [BIG[:, 12288 + k * 512:12288 + (k + 1) * 512] for k in range(8)]
        tq = [BIG[:, 8192 + k * 512:8192 + (k + 1) * 512] for k in range(8)]
        T8f_ = self.T8[:].rearrange("p a f -> p (a f)")
        def zbuf(base_ap, k):
            return base_ap[:, k * 384:(k + 1) * 384]
        ZB = [[[zbuf(PH[:, 2048:4096], 0), zbuf(PH[:, 2048:4096], 1)], [zbuf(PH[:, 2048:4096], 2), zbuf(PH[:, 2048:4096], 3)]],
              [[zbuf(T8f_, 0), zbuf(T8f_, 1)], [zbuf(T8f_, 2), zbuf(T8f_, 3)]]]
        SSM = [[[b[:, 128:384] for b in pair] for pair in st_] for st_ in ZB]
        coef = PH[:, 1024:1744].rearrange("p (k c j) -> p k c j", k=15, c=3)
        lamp = PH[:, 1744:1792].rearrange("p (r j) -> p r j", r=3)
        tiny = [PH[:, 1792 + 16 * k:1808 + 16 * k] for k in range(12)]
        dcol = PH[:, 1984:1988]
        BBs = [Y16[:, 0:2048].rearrange("p (j m) -> p j m", j=16), Y16[:, 2048:4096].rearrange("p (j m) -> p j m", j=16)]
        CCb = [Y16[:, 4096:6144].rearrange("p (j m) -> p j m", j=16), Y16[:, 6144:8192].rearrange("p (j m) -> p j m", j=16)]
        slots = [self.load_w(dr["w_in"][l, :, UOFF + 256 * b:UOFF + 256 * (b + 1)], 8, 256) for b in range(2)]
        bank = 0
        for b in range(2):
            for tt in range(4):
                for o2 in range(2):
                    oc = 2 * b + o2
                    bk = bank % 8
                    bank += 1
                    for c in range(8):
                        S.pe(lambda e, c=c, bk=bk, b=b, o2=o2, tt=tt: e.matmul(self.P[:, bk, :], lhsT=self.wsl[:, slots[b], c, o2 * 128:(o2 + 1) * 128],
                                                                          rhs=self.hT[:, c, tt * 512:(tt + 1) * 512], start=(c == 0), stop=(c == 7)),
                             reads=[f"w{slots[b]}"] + [f"hT{4 * tt + k}" for k in range(4)], writes=[f"ps{bk}"])
                    S.act(lambda e, bk=bk, oc=oc, tt=tt: e.activation(out=uz[:, oc, tt * 512:(tt + 1) * 512], in_=self.P[:, bk, :], func=AF.Copy),
                          writes=[f"ps{bk}", f"uz{oc}_{tt}"])
        self.dump("uT", uz[:], [128, 4, SEQ], [f"uz{oc}_{tt}" for oc in range(4) for tt in range(4)])
        S.dma("sp", lambda e: e.dma_start(out=lamp, in_=dr["s5_lamp"][l]), writes=["pl_lre", "pl_lim", "pl_lst"], key="lamp")
        S.dma("sp", lambda e: e.dma_start(out=dcol, in_=dr["s5_dcol"][l]), writes=["dcol"], key="dcol")
        ccf = [BIG[:, 12288:14336].rearrange("p (j m) -> p j m", j=16), BIG[:, 14336:16384].rearrange("p (j m) -> p j m", j=16)]
        for comp in range(2):
            S.dma("pool", lambda e, comp=comp: e.dma_start(out=BBs[comp], in_=dr["s5_BB"][l, comp]), writes=[f"BBs{comp}"], key=f"BBs{comp}")
            S.dma("sp", lambda e, comp=comp: e.dma_start(out=ccf[comp], in_=dr["s5_CC"][l, comp]), writes=[f"ccf{comp}"], key=f"ccf{comp}")
        are, aim = self.abar_tables(lamp[:, 0, :], lamp[:, 1, :], lamp[:, 2, :], tiny[0:6], "pl")
        pw = {}
        S.dve(lambda e: e.tensor_copy(out=coef[:, 0, 0, :], in_=are), reads=["pl_t0"], writes=["pw"])
        S.dve(lambda e: e.tensor_copy(out=coef[:, 0, 1, :], in_=aim), reads=["pl_t1"], writes=["pw"])
        for idx in range(1, 15):
            if POWS[idx] <= 8:
                a_i, b_i = idx - 1, 0
            else:
                a_i, b_i = idx - 1, idx - 1
            self.cmul(coef[:, idx, 0, :], coef[:, idx, 1, :], coef[:, a_i, 0, :], coef[:, a_i, 1, :], coef[:, b_i, 0, :], coef[:, b_i, 1, :], tiny[6:8], "pw")
        for idx in range(15):
            S.dve(lambda e, idx=idx: e.tensor_scalar(out=coef[:, idx, 2, :], in0=coef[:, idx, 1, :], scalar1=-1.0, scalar2=None, op0=ALU.mult), reads=["pw"], writes=["pw"])
        lr, li = lamp[:, 0, :], lamp[:, 1, :]
        z = tiny[8:12]
        S.dve(lambda e: e.tensor_tensor(out=z[0], in0=lr, in1=lr, op=ALU.mult), reads=["pl_lre"], writes=["z0"])
        S.dve(lambda e: e.tensor_tensor(out=z[1], in0=li, in1=li, op=ALU.mult), reads=["pl_lim"], writes=["z1"])
        S.dve(lambda e: e.tensor_tensor(out=z[0], in0=z[0], in1=z[1], op=ALU.add), reads=["z0", "z1"], writes=["z0"])
        S.dve(lambda e: e.reciprocal(out=z[0], in_=z[0]), reads=["z0"], writes=["z0"])
        S.dve(lambda e: e.tensor_scalar(out=z[1], in0=are, scalar1=-1.0, scalar2=None, op0=ALU.add), reads=["pl_t0", "z0"], writes=["z1"])
        S.dve(lambda e: e.tensor_tensor(out=z[2], in0=z[1], in1=lr, op=ALU.mult), reads=["z1", "pl_lre"], writes=["z2"])
        S.dve(lambda e: e.tensor_tensor(out=z[3], in0=aim, in1=li, op=ALU.mult), reads=["pl_t1", "pl_lim"], writes=["z3"])
        S.dve(lambda e: e.tensor_tensor(out=z[2], in0=z[2], in1=z[3], op=ALU.add), reads=["z2", "z3"], writes=["z2"])
        S.dve(lambda e: e.tensor_tensor(out=z[2], in0=z[2], in1=z[0], op=ALU.mult), reads=["z2", "z0"], writes=["z2"])
        S.dve(lambda e: e.tensor_tensor(out=z[3], in0=aim, in1=lr, op=ALU.mult), reads=["pl_t1", "pl_lre", "z2"], writes=["z3"])
        S.dve(lambda e: e.tensor_tensor(out=z[1], in0=z[1], in1=li, op=ALU.mult), reads=["z1", "pl_lim"], writes=["z1"])
        S.dve(lambda e: e.tensor_tensor(out=z[3], in0=z[3], in1=z[1], op=ALU.subtract), reads=["z3", "z1"], writes=["z3"])
        S.dve(lambda e: e.tensor_tensor(out=z[3], in0=z[3], in1=z[0], op=ALU.mult), reads=["z3", "z0"], writes=["z3"])
        S.dve(lambda e: e.tensor_scalar(out=z[1], in0=z[3], scalar1=-1.0, scalar2=None, op0=ALU.mult), reads=["z3"], writes=["z1"])
        tmpc = self.T8[:].rearrange("p a f -> p (a f)")[:, 0:128]
        for j in range(16):
            zr, zi, nzi = z[2][:, j:j + 1], z[3][:, j:j + 1], z[1][:, j:j + 1]
            S.dve(lambda e, j=j, zr=zr: e.tensor_scalar(out=tmpc, in0=ccf[0][:, j, :], scalar1=zr, scalar2=None, op0=ALU.mult), reads=["ccf0", "z2"], writes=["tmpc"])
            S.dve(lambda e, j=j, nzi=nzi: e.scalar_tensor_tensor(out=CCb[0][:, j, :], in0=ccf[1][:, j, :], scalar=nzi, in1=tmpc, op0=ALU.mult, op1=ALU.add), reads=["ccf1", "z1", "tmpc"], writes=["CC0"])
            S.dve(lambda e, j=j, zi=zi: e.tensor_scalar(out=tmpc, in0=ccf[0][:, j, :], scalar1=zi, scalar2=None, op0=ALU.mult), reads=["ccf0", "z3", "CC0"], writes=["tmpc"])
            S.dve(lambda e, j=j, zr=zr: e.scalar_tensor_tensor(out=CCb[1][:, j, :], in0=ccf[1][:, j, :], scalar=zr, in1=tmpc, op0=ALU.mult, op1=ALU.add), reads=["ccf1", "z2", "tmpc"], writes=["CC1"])
        S.barrier()
        S.dve(lambda e: e.memset(PH[:, 2048:2048 + 1536], 0.0), writes=["zpad0"])
        S.dve(lambda e: e.memset(T8f_[:, 0:1536], 0.0), writes=["zpad1"])
        v8 = lambda ap: ap.rearrange("p (b s) -> p b s", s=8)
        t8 = lambda ap: ap.rearrange("p (s b) -> p s b", s=8)
        nbank = [0]
        l3t = [PH[:, 3072 + 256 * k:3072 + 256 * (k + 1)] for k in range(4)]

        def stageX(j):
            q, sx = j // 4, j % 2
            A, Ssm = SA[sx], SSM[sx]
            cn = f"sA{sx}_"
            for tt in range(4):
                for comp in range(2):
                    bk = 4 + nbank[0] % 4
                    nbank[0] += 1
                    S.pe(lambda e, bk=bk, comp=comp, tt=tt: e.matmul(self.P[:, bk, :], lhsT=BBs[comp][:, j, :], rhs=uz[:, q, tt * 512:(tt + 1) * 512], start=True, stop=True),
                         reads=[f"BBs{comp}", f"uz{q}_{tt}"], writes=[f"ps{bk}"])
                    S.act(lambda e, bk=bk, comp=comp, tt=tt: e.activation(out=t8(A[comp])[:, :, tt * 64:(tt + 1) * 64].rearrange("p s b -> p b s"), in_=self.P[:, bk, :].rearrange("p (b s) -> p b s", s=8), func=AF.Copy),
                          writes=[f"ps{bk}", f"{cn}{comp}"])

        def stageXd(j):
            q, sx = j // 4, j % 2
            A, Ssm = SA[sx], SSM[sx]
            cn = f"sA{sx}_"
            idx1 = POWS.index(1)
            car, cai, cnai = coef[:, idx1, 0, j:j + 1], coef[:, idx1, 1, j:j + 1], coef[:, idx1, 2, j:j + 1]
            a_re, a_im = t8(A[0]), t8(A[1])
            for tau in range(1, 8):
                S.dve(lambda e, tau=tau: e.scalar_tensor_tensor(out=a_re[:, tau, :], in0=a_re[:, tau - 1, :], scalar=car, in1=a_re[:, tau, :], op0=ALU.mult, op1=ALU.add),
                      reads=[f"{cn}0", "pw"], writes=[f"{cn}0"])
                S.dve(lambda e, tau=tau: e.scalar_tensor_tensor(out=a_im[:, tau, :], in0=a_im[:, tau - 1, :], scalar=car, in1=a_im[:, tau, :], op0=ALU.mult, op1=ALU.add),
                      reads=[f"{cn}1", "pw"], writes=[f"{cn}1"])
                S.dve(lambda e, tau=tau: e.scalar_tensor_tensor(out=a_re[:, tau, :], in0=a_im[:, tau - 1, :], scalar=cnai, in1=a_re[:, tau, :], op0=ALU.mult, op1=ALU.add),
                      reads=[f"{cn}0", f"{cn}1", "pw"], writes=[f"{cn}0"])
                S.dve(lambda e, tau=tau: e.scalar_tensor_tensor(out=a_im[:, tau, :], in0=a_re[:, tau - 1, :], scalar=cai, in1=a_im[:, tau, :], op0=ALU.mult, op1=ALU.add),
                      reads=[f"{cn}0", f"{cn}1", "pw"], writes=[f"{cn}1"])
            s0, s1, n0, n1 = Ssm[0], Ssm[1], f"sS0{sx}_", f"sS1{sx}_"
            for comp in range(2):
                S.act(lambda e, comp=comp, s0=s0: e.activation(out=s0[comp], in_=t8(A[comp])[:, 7, :], func=AF.Copy),
                      reads=[f"{cn}{comp}", f"zpad{sx}"], writes=[f"{n0}{comp}"])
            zb0, zb1 = ZB[sx][0], ZB[sx][1]
            for d in (1, 2, 4, 8, 16, 32, 64, 128):
                idx = POWS.index(8 * d)
                c_r, c_i, c_ni = coef[:, idx, 0, j:j + 1], coef[:, idx, 1, j:j + 1], coef[:, idx, 2, j:j + 1]
                sh = slice(128 - d, 384 - d)
                S.dve(lambda e, zb0=zb0, s0=s0, s1=s1, sh=sh, c_r=c_r: e.scalar_tensor_tensor(out=s1[0], in0=zb0[0][:, sh], scalar=c_r, in1=s0[0], op0=ALU.mult, op1=ALU.add),
                      reads=[f"{n0}0", "pw", f"zpad{sx}"], writes=[f"{n1}0"])
                S.dve(lambda e, zb0=zb0, s0=s0, s1=s1, sh=sh, c_r=c_r: e.scalar_tensor_tensor(out=s1[1], in0=zb0[1][:, sh], scalar=c_r, in1=s0[1], op0=ALU.mult, op1=ALU.add),
                      reads=[f"{n0}1", "pw", f"zpad{sx}"], writes=[f"{n1}1"])
                S.dve(lambda e, zb0=zb0, s1=s1, sh=sh, c_ni=c_ni: e.scalar_tensor_tensor(out=s1[0], in0=zb0[1][:, sh], scalar=c_ni, in1=s1[0], op0=ALU.mult, op1=ALU.add),
                      reads=[f"{n0}1", f"{n1}0", "pw"], writes=[f"{n1}0"])
                S.dve(lambda e, zb0=zb0, s1=s1, sh=sh, c_i=c_i: e.scalar_tensor_tensor(out=s1[1], in0=zb0[0][:, sh], scalar=c_i, in1=s1[1], op0=ALU.mult, op1=ALU.add),
                      reads=[f"{n0}0", f"{n1}1", "pw"], writes=[f"{n1}1"])
                s0, s1, n0, n1 = s1, s0, n1, n0
                zb0, zb1 = zb1, zb0

        def stageY(j):
            q, sx, hbuf = j // 4, j % 2, j % 2
            A, Ssm = SA[sx], SSM[sx]
            cn = f"sA{sx}_"
            s0, n0 = Ssm[0], f"sS0{sx}_"
            rr = [f"{n0}0", f"{n0}1", "pw"]
            for tau in range(8):
                car, cai, cnai = coef[:, tau, 0, j:j + 1], coef[:, tau, 1, j:j + 1], coef[:, tau, 2, j:j + 1]
                dre, dim = t8(A[0])[:, tau, 1:], t8(A[1])[:, tau, 1:]
                S.dve(lambda e, dre=dre, car=car: e.scalar_tensor_tensor(out=dre, in0=s0[0][:, 0:255], scalar=car, in1=dre, op0=ALU.mult, op1=ALU.add), reads=rr + [f"{cn}0"], writes=[f"{cn}0"])
                S.dve(lambda e, dim=dim, car=car: e.scalar_tensor_tensor(out=dim, in0=s0[1][:, 0:255], scalar=car, in1=dim, op0=ALU.mult, op1=ALU.add), reads=rr + [f"{cn}1"], writes=[f"{cn}1"])
                S.dve(lambda e, dre=dre, cnai=cnai: e.scalar_tensor_tensor(out=dre, in0=s0[1][:, 0:255], scalar=cnai, in1=dre, op0=ALU.mult, op1=ALU.add), reads=rr + [f"{cn}0"], writes=[f"{cn}0"])
                S.dve(lambda e, dim=dim, cai=cai: e.scalar_tensor_tensor(out=dim, in0=s0[0][:, 0:255], scalar=cai, in1=dim, op0=ALU.mult, op1=ALU.add), reads=rr + [f"{cn}1"], writes=[f"{cn}1"])
            S.act(lambda e: e.activation(out=hb[:, hbuf, 0, :].rearrange("p (b s) -> p b s", s=8), in_=t8(A[0]).rearrange("p s b -> p b s"), func=AF.Copy), reads=[f"{cn}0"], writes=[f"hb{hbuf}"])
            S.act(lambda e: e.activation(out=hb[:, hbuf, 1, :].rearrange("p (b s) -> p b s", s=8), in_=t8(A[1]).rearrange("p s b -> p b s"), func=AF.Identity, scale=-1.0), reads=[f"{cn}1"], writes=[f"hb{hbuf}"])
            for tt in range(4):
                for comp in range(2):
                    S.pe(lambda e, tt=tt, comp=comp: e.matmul(self.P[:, tt, :], lhsT=CCb[comp][:, j, :], rhs=hb[:, hbuf, comp, tt * 512:(tt + 1) * 512],
                                                              start=(j == 4 * q and comp == 0), stop=(j == 4 * q + 3 and comp == 1)),
                         reads=[f"CC{comp}", f"hb{hbuf}"], writes=[f"ps{tt}"])
            if j % 4 != 3:
                return
            for tt in range(4):
                ya, yb = tp[2 * (tt % 2)], tp[2 * (tt % 2) + 1]
                na, nb = f"gl{2 * (tt % 2)}", f"gl{2 * (tt % 2) + 1}"
                usl = uz[:, q, tt * 512:(tt + 1) * 512]
                S.dve(lambda e, ya=ya, usl=usl, tt=tt: e.scalar_tensor_tensor(out=ya, in0=usl, scalar=dcol[:, q:q + 1], in1=self.P[:, tt, :], op0=ALU.mult, op1=ALU.add),
                      reads=[f"uz{q}_{tt}", "dcol"], writes=[f"ps{tt}", na])
                S.act(lambda e, ya=ya, yb=yb: e.activation(out=yb, in_=ya, func=AF.Square), reads=[na], writes=[nb])
                S.act(lambda e, yb=yb: e.activation(out=yb, in_=yb, func=AF.Identity, scale=0.044715, bias=1.0), reads=[nb], writes=[nb])
                S.dve(lambda e, ya=ya, yb=yb: e.tensor_tensor(out=yb, in0=yb, in1=ya, op=ALU.mult), reads=[na, nb], writes=[nb])
                S.act(lambda e, yb=yb: e.activation(out=yb, in_=yb, func=AF.Sigmoid, scale=1.5957691216057308), reads=[nb], writes=[nb])
                S.dve(lambda e, ya=ya, yb=yb, usl=usl: e.tensor_tensor(out=usl, in0=ya, in1=yb, op=ALU.mult), reads=[na, nb], writes=[f"uz{q}_{tt}"])

        stageX(0)
        stageXd(0)
        for j in range(16):
            if j + 1 < 16:
                stageX(j + 1)
            stageY(j)
            if j + 1 < 16:
                stageXd(j + 1)
        self.dump("zT", uz[:], [128, 4, SEQ], [f"uz{oc}_{tt}" for oc in range(4) for tt in range(4)])
        S.barrier()
        gs = [self.load_w(dr["w_glu"][l, :, 256 * b:256 * (b + 1)], 4, 256) for b in range(2)]
        for tt in range(4):
            for oc in range(4):
                bk = 4 * (tt % 2) + oc
                for kc in range(4):
                    S.pe(lambda e, bk=bk, oc=oc, kc=kc, tt=tt: e.matmul(self.P[:, bk, :], lhsT=self.wsl[:, gs[oc // 2], kc, (oc % 2) * 128:(oc % 2 + 1) * 128],
                                                                    rhs=uz[:, kc, tt * 512:(tt + 1) * 512], start=(kc == 0), stop=(kc == 3)),
                         reads=[f"w{gs[oc // 2]}"] + [f"uz{k}_{tt}" for k in range(4)], writes=[f"ps{bk}"])
            for oc in range(4):
                bk = 4 * (tt % 2) + oc
                sg = self.T8[:, oc, :]
                S.act(lambda e, bk=bk, sg=sg: e.activation(out=sg, in_=self.P[:, bk, :], func=AF.Sigmoid), writes=[f"ps{bk}", f"T8_{oc}"])
                S.dve(lambda e, sg=sg, oc=oc, tt=tt: e.tensor_tensor(out=uz[:, oc, tt * 512:(tt + 1) * 512], in0=uz[:, oc, tt * 512:(tt + 1) * 512], in1=sg, op=ALU.mult),
                      reads=[f"T8_{oc}", f"uz{oc}_{tt}"], writes=[f"uz{oc}_{tt}"])
        self.dump("z2T", uz[:], [128, 4, SEQ], [f"uz{oc}_{tt}" for oc in range(4) for tt in range(4)])


    def big_views(self):
        Bb = self.BIG[:].bitcast(BF16)
        v = {}
        v["qT"] = Bb[:, 0:4096].rearrange("p (c t) -> p c t", c=4)
        v["kT"] = Bb[:, 4096:8192].rearrange("p (c t) -> p c t", c=4)
        v["qdT"] = Bb[:, 8192:12288].rearrange("p (c t) -> p c t", c=4)
        v["v_tm"] = Bb[:, 12288:20480].rearrange("p (t d) -> p t d", t=8)
        v["rT"] = Bb[:, 20480:28672].rearrange("p (c t) -> p c t", c=8)
        v["mergedT"] = Bb[:, 0:8192].rearrange("p (c t) -> p c t", c=8)
        v["rt"] = [self.BIG[:, 14336 + k * 512:14336 + (k + 1) * 512] for k in range(4)]
        return v

    def wbig(self, k):
        return self.wsl[:, 2 * k:2 * k + 2, :, :].rearrange("p s c n -> p (s c n)").rearrange("p (c n) -> p c n", c=8)

    def load_wbig(self, src2d, kc, k):
        dst = self.wbig(k)[:, 0:kc, :]
        src = src2d.rearrange("(c p) n -> p c n", p=128)
        self.S.dma("pool", lambda e: e.dma_start(out=dst, in_=src), writes=[f"w{2 * k}", f"w{2 * k + 1}"], key=f"wb{k}", nobar=True)

    def phase_ret(self, l, half):
        S, dr, P = self.S, self.dr, self.P
        V = self.big_views()
        qT, kT, qdT, v_tm, rT, rt = V["qT"], V["kT"], V["qdT"], V["v_tm"], V["rT"], V["rt"]
        PH, T8 = self.PH, self.T8
        rope = PH[:, 0:2048].rearrange("p (a t) -> p a t", a=2)
        ktm = PH[:, 2048:3072].bitcast(BF16).rearrange("p (b e f) -> p b e f", b=2, e=2)
        ST = PH[:, 3072:3584].bitcast(BF16).rearrange("p (h i) -> p h i", h=8)
        sgt = PH[:, 3584:4096].bitcast(BF16).rearrange("p (b f) -> p b f", b=2)
        tok0 = half * 1024
        S32, Sbf = self.S32, self.Sbf
        if half == 0:
            S.pool(lambda e: e.memset(S32[:], 0.0), writes=["S32"])
            S.pool(lambda e: e.memset(Sbf[:, 0, :], 0.0), writes=["Sbf0"])
        for a in range(2):
            S.dma("sp", lambda e, a=a: e.dma_start(out=rope[:, a, :], in_=dr["c_rope"][a, :, tok0:tok0 + 1024]), writes=[f"rope{a}"])
        nb = 0
        for which, off, swoff, dst, nm in (("q", QOFF, 0, qT, "qT"), ("k", KOFF, 512, kT, "kT")):
            for b in range(2):
                sm = self.load_w(dr["w_in"][l, :, off + 256 * b:off + 256 * (b + 1)], 8, 256)
                ss = self.load_w(dr["w_qksw"][l, :, swoff + 256 * b:swoff + 256 * (b + 1)], 8, 256)
                for tt in range(2):
                    for o2 in range(2):
                        oc = 2 * b + o2
                        ba, bb_ = 2 * (nb % 4), 2 * (nb % 4) + 1
                        nb += 1
                        hts = [f"hT{8 * half + 4 * tt + k}" for k in range(4)]
                        g0 = tok0 + tt * 512
                        for c in range(8):
                            S.pe(lambda e, c=c, ba=ba, sm=sm, o2=o2, g0=g0: e.matmul(P[:, ba, :], lhsT=self.wsl[:, sm, c, o2 * 128:(o2 + 1) * 128], rhs=self.hT[:, c, g0:g0 + 512], start=(c == 0), stop=(c == 7)),
                                 reads=[f"w{sm}"] + hts, writes=[f"ps{ba}"])
                        for c in range(8):
                            S.pe(lambda e, c=c, bb_=bb_, ss=ss, o2=o2, g0=g0: e.matmul(P[:, bb_, :], lhsT=self.wsl[:, ss, c, o2 * 128:(o2 + 1) * 128], rhs=self.hT[:, c, g0:g0 + 512], start=(c == 0), stop=(c == 7)),
                                 reads=[f"w{ss}"] + hts, writes=[f"ps{bb_}"])
                        lsl = slice(tt * 512, (tt + 1) * 512)
                        r0, r1 = rt[2 * (nb % 2)], rt[2 * (nb % 2) + 1]
                        n0_, n1_ = f"rt{2 * (nb % 2)}", f"rt{2 * (nb % 2) + 1}"
                        S.dve(lambda e, ba=ba, lsl=lsl, r0=r0: e.tensor_tensor(out=r0, in0=P[:, ba, :], in1=rope[:, 0, lsl], op=ALU.mult), reads=["rope0"], writes=[f"ps{ba}", n0_])
                        S.dve(lambda e, bb_=bb_, lsl=lsl, r1=r1: e.tensor_tensor(out=r1, in0=P[:, bb_, :], in1=rope[:, 1, lsl], op=ALU.mult), reads=["rope1"], writes=[f"ps{bb_}", n1_])
                        S.dve(lambda e, dst=dst, oc=oc, lsl=lsl, r0=r0, r1=r1: e.tensor_tensor(out=dst[:, oc, lsl], in0=r0, in1=r1, op=ALU.add), reads=[n0_, n1_], writes=[f"{nm}{oc}_{tt}"])
                        if which == "q":
                            S.dve(lambda e, oc=oc, lsl=lsl: e.tensor_tensor(out=qdT[:, oc, lsl].rearrange("p (n i) -> p n i", i=64), in0=qT[:, oc, lsl].rearrange("p (n i) -> p n i", i=64),
                                                                         in1=self.qdec[:, oc, :].unsqueeze(1).broadcast_to([128, 8, 64]), op=ALU.mult),
                                   reads=[f"qT{oc}_{tt}", "c_qdec"], writes=[f"qdT{oc}_{tt}"])
        self.dump("qT", qT, [128, 4, 1024], [f"qT{oc}_{tt}" for oc in range(4) for tt in range(2)])
        self.dump("kT", kT, [128, 4, 1024], [f"kT{oc}_{tt}" for oc in range(4) for tt in range(2)])
        if DEBUG["stop"] == "retqk":
            return
        for vb in range(2):
            self.load_wbig(dr["w_in"][l, :, VOFF + 512 * vb:VOFF + 512 * (vb + 1)], 8, vb)
        nb = 0
        for tl in range(8):
            T = 8 * half + tl
            for vb in range(2):
                bk = nb % 8
                nb += 1
                for c in range(8):
                    S.pe(lambda e, c=c, bk=bk, vb=vb, T=T: e.matmul(P[:, bk, :], lhsT=self.hT[:, c, T * 128:(T + 1) * 128], rhs=self.wbig(vb)[:, c, :], start=(c == 0), stop=(c == 7)),
                         reads=[f"w{2 * vb}", f"w{2 * vb + 1}", f"hT{T}"], writes=[f"ps{bk}"])
                S.act(lambda e, bk=bk, tl=tl, vb=vb: e.activation(out=v_tm[:, tl, vb * 512:(vb + 1) * 512], in_=P[:, bk, :], func=AF.Copy), writes=[f"ps{bk}", f"v{tl}"])
        self.dump("v_tm", v_tm, [128, 8, 1024], [f"v{tl}" for tl in range(8)])
        nb = 0
        for b in range(4):
            sg_ = self.load_w(dr["w_in"][l, :, GOFF + 256 * b:GOFF + 256 * (b + 1)], 8, 256)
            for tt in range(2):
                for o2 in range(2):
                    oc = 2 * b + o2
                    bk = nb % 8
                    nb += 1
                    g0 = tok0 + tt * 512
                    for c in range(8):
                        S.pe(lambda e, c=c, bk=bk, sg_=sg_, o2=o2, g0=g0: e.matmul(P[:, bk, :], lhsT=self.wsl[:, sg_, c, o2 * 128:(o2 + 1) * 128], rhs=self.hT[:, c, g0:g0 + 512], start=(c == 0), stop=(c == 7)),
                             reads=[f"w{sg_}"] + [f"hT{8 * half + 4 * tt + k}" for k in range(4)], writes=[f"ps{bk}"])
                    S.act(lambda e, bk=bk, oc=oc, tt=tt: e.activation(out=rT[:, oc, tt * 512:(tt + 1) * 512], in_=P[:, bk, :], func=AF.Silu), writes=[f"ps{bk}"] + [f"rT{4 * tt + k}" for k in range(4)])

        def stageA(tl):
            T = 8 * half + tl
            pb = tl % 2
            tsl = slice(tl * 128, (tl + 1) * 128)
            Pb0 = P[:, 0, :].bitcast(BF16)
            for c4 in range(4):
                S.pe(lambda e, c4=c4: e.transpose(out=Pb0[:, c4 * 128:(c4 + 1) * 128], in_=kT[:, c4, tsl], identity=self.ident_b[:]),
                     reads=[f"kT{c4}_{tl // 4}", "ident_b"], writes=["ps0"])
            for eo in range(2):
                S.dve(lambda e, eo=eo: e.tensor_tensor(out=ktm[:, pb, eo, :].rearrange("p (h d) -> p h d", h=8), in0=Pb0[:, 0:512].rearrange("p (h d) -> p h d", h=8),
                                                       in1=self.kdec[:, eo, :].unsqueeze(2).broadcast_to([128, 8, 64]), op=ALU.mult),
                      reads=["c_kdec"], writes=["ps0", f"ktm{pb}_{eo}"])
            for eo in range(2):
                for h in range(8):
                    c4 = h // 2
                    S.pe(lambda e, h=h, c4=c4, eo=eo: e.matmul(P[:, 1 + h // 4, (h % 4) * 128:(h % 4 + 1) * 128], lhsT=ktm[:, pb, eo, c4 * 128:(c4 + 1) * 128],
                                                              rhs=v_tm[:, tl, h * 128:(h + 1) * 128], start=True, stop=True),
                         reads=[f"ktm{pb}_{eo}", f"v{tl}"], writes=[f"ps{1 + h // 4}"])
                tmp = T8[:, 3, :]
                S.pool(lambda e: e.tensor_tensor(out=S32[:].rearrange("p (h e) -> p h e", h=8), in0=S32[:].rearrange("p (h e) -> p h e", h=8),
                                                 in1=self.sdec[:].unsqueeze(2).broadcast_to([128, 8, 128]), op=ALU.mult), reads=["S32", "c_sdec"], writes=["S32"])
                for k in range(2):
                    S.dve(lambda e, k=k: e.tensor_tensor(out=S32[:, k * 512:(k + 1) * 512], in0=S32[:, k * 512:(k + 1) * 512], in1=P[:, 1 + k, :], op=ALU.add),
                          reads=["S32"], writes=["S32", f"ps{1 + k}"])
                if eo == 0:
                    idx = 3 + T % 2
                else:
                    idx = (T + 1) % 3
                S.act(lambda e, idx=idx: e.activation(out=Sbf[:, idx, :], in_=S32[:], func=AF.Copy), reads=["S32"], writes=[f"Sbf{idx}"])

        STs = [ST, PH[:, 3584:4096].bitcast(BF16).rearrange("p (h i) -> p h i", h=8)]

        def stageBf(tl):
            tsl = slice(tl * 128, (tl + 1) * 128)
            STb = STs[tl % 2]
            ST4 = STb.rearrange("p (c hh) i -> p c hh i", hh=2)
            in4 = self.intra[:].rearrange("p (c hh) i -> p c hh i", hh=2)
            for h in range(8):
                c4, hh = h // 2, h % 2
                S.pe(lambda e, c4=c4, hh=hh: e.matmul(P[:, 3 + hh, c4 * 128:(c4 + 1) * 128], lhsT=kT[hh * 64:(hh + 1) * 64, c4, tsl], rhs=qT[hh * 64:(hh + 1) * 64, c4, tsl], start=True, stop=True),
                     reads=[f"kT{c4}_{tl // 4}", f"qT{c4}_{tl // 4}"], writes=[f"ps{3 + hh}"])
            for hh in range(2):
                S.dve(lambda e, hh=hh: e.tensor_tensor(out=ST4[:, :, hh, :], in0=P[:, 3 + hh, :].rearrange("p (c i) -> p c i", c=4), in1=in4[:, :, hh, :], op=ALU.mult),
                      reads=["c_intra"], writes=[f"ps{3 + hh}", f"ST{tl % 2}_{hh}"])

        def stageBb(tl):
            T = 8 * half + tl
            tsl = slice(tl * 128, (tl + 1) * 128)
            ie, io = T % 3, 3 + T % 2
            STb = STs[tl % 2]
            rT4 = rT.rearrange("p (c hh) t -> p c hh t", hh=2)
            for h in range(8):
                c4, hh = h // 2, h % 2
                ob = P[:, 5 + hh, c4 * 128:(c4 + 1) * 128]
                S.pe(lambda e, h=h, ob=ob: e.matmul(ob, lhsT=v_tm[:, tl, h * 128:(h + 1) * 128], rhs=STb[:, h, :], start=True, stop=False),
                     reads=[f"v{tl}", f"ST{tl % 2}_{hh}"], writes=[f"ps{5 + hh}"])
                S.pe(lambda e, h=h, ob=ob, c4=c4, hh=hh: e.matmul(ob[:, 0:64], lhsT=Sbf[hh * 64:(hh + 1) * 64, ie, h * 128:(h + 1) * 128], rhs=qdT[hh * 64:(hh + 1) * 64, c4, tl * 128:tl * 128 + 64], start=False, stop=False),
                     reads=[f"Sbf{ie}", f"qdT{c4}_{tl // 4}"], writes=[f"ps{5 + hh}"])
                S.pe(lambda e, h=h, ob=ob, c4=c4, hh=hh: e.matmul(ob[:, 64:128], lhsT=Sbf[hh * 64:(hh + 1) * 64, io, h * 128:(h + 1) * 128], rhs=qdT[hh * 64:(hh + 1) * 64, c4, tl * 128 + 64:tl * 128 + 128], start=False, stop=True),
                     reads=[f"Sbf{io}", f"qdT{c4}_{tl // 4}"], writes=[f"ps{5 + hh}"])
            for hh in range(2):
                bo, bm, bq = 5 + hh, 7, 5 + hh
                o_sb, osq, w2 = (T8[:, 0, :], T8[:, 1, :], T8[:, 2, :]) if hh == 0 else (rt[0], rt[1], rt[2])
                n0, n1, n2 = (f"hn{hh}a", f"hn{hh}b", f"hn{hh}c") if hh == 0 else ("rt0", "rt1", "rt2")
                S.act(lambda e, bo=bo, o_sb=o_sb: e.activation(out=o_sb, in_=P[:, bo, :], func=AF.Copy), writes=[f"ps{bo}", n0])
                S.act(lambda e, bo=bo, osq=osq: e.activation(out=osq, in_=P[:, bo, :], func=AF.Square), writes=[f"ps{bo}", n1])
                S.pe(lambda e, o_sb=o_sb: e.matmul(P[:, bm, :], lhsT=self.ones_f[:], rhs=o_sb, start=True, stop=True), reads=["c_ones", n0], writes=[f"ps{bm}"])
                S.pe(lambda e, osq=osq, bq=bq: e.matmul(P[:, bq, :], lhsT=self.ones_f[:], rhs=osq, start=True, stop=True), reads=["c_ones", n1], writes=[f"ps{bq}"])
                S.act(lambda e, osq=osq: e.activation(out=osq, in_=P[:, bm, :], func=AF.Copy), reads=[n1], writes=[f"ps{bm}", n1])
                S.act(lambda e, w2=w2: e.activation(out=w2, in_=P[:, bm, :], func=AF.Square), writes=[f"ps{bm}", n2])
                S.dve(lambda e, w2=w2, bq=bq: e.tensor_tensor(out=w2, in0=P[:, bq, :], in1=w2, op=ALU.subtract), reads=[n2], writes=[f"ps{bq}", n2])
                S.act(lambda e, w2=w2: e.activation(out=w2, in_=w2, func=AF.Sqrt, bias=HN_EPS, scale=1.0), reads=[n2], writes=[n2])
                S.dve(lambda e, w2=w2: e.reciprocal(out=w2, in_=w2), reads=[n2], writes=[n2])
                S.dve(lambda e, o_sb=o_sb, osq=osq: e.tensor_tensor(out=o_sb, in0=o_sb, in1=osq, op=ALU.subtract), reads=[n0, n1], writes=[n0])
                S.dve(lambda e, o_sb=o_sb, w2=w2: e.tensor_tensor(out=o_sb, in0=o_sb, in1=w2, op=ALU.mult), reads=[n0, n2], writes=[n0])
                S.dve(lambda e, o_sb=o_sb, hh=hh: e.tensor_tensor(out=rT4[:, :, hh, tsl], in0=o_sb.rearrange("p (c i) -> p c i", c=4), in1=rT4[:, :, hh, tsl], op=ALU.mult),
                      reads=[n0, f"rT{tl}"], writes=[f"rT{tl}"])

        stageA(0)
        stageBf(0)
        for tl in range(8):
            if tl + 1 < 8:
                stageA(tl + 1)
                stageBf(tl + 1)
            stageBb(tl)
        self.dump("onT", rT, [128, 8, 1024], [f"rT{tl}" for tl in range(8)])
        if DEBUG["stop"] == "retr":
            return
        self.dump("rT", rT, [128, 8, 1024], [f"rT{tl}" for tl in range(8)])


    def phase_tail(self, l, half):
        S, dr, P = self.S, self.dr, self.P
        V = self.big_views()
        rT, mergedT, rt = V["rT"], V["mergedT"], V["rt"]
        uz = self.uz
        tok0 = half * 1024
        self.load_ln_params(1 + l)
        if half == 0:
            S.dma("sp", lambda e: e.dma_start(out=self.wr_f[:], in_=dr["w_router"][l].rearrange("(c p) n -> p c n", p=128)), writes=["wr_f"])
            S.dma("sp", lambda e: e.dma_start(out=self.rb[:], in_=dr["b_router"][l].partition_broadcast(128)), writes=["rb"])
        step = 0
        for bb in range(4):
            s_gr = self.load_w(dr["w_in"][l, :, GROFF + 256 * bb:GROFF + 256 * (bb + 1)], 8, 256)
            s_gs = self.load_w(dr["w_in"][l, :, GSOFF + 256 * bb:GSOFF + 256 * (bb + 1)], 8, 256)
            s_br = self.load_w(dr["w_branch_ret"][l, :, 256 * bb:256 * (bb + 1)], 8, 256)
            s_bs = self.load_w(dr["w_branch_s5"][l, :, 256 * bb:256 * (bb + 1)], 4, 256)
            for tt in range(2):
                g0 = tok0 + tt * 512
                lsl = slice(tt * 512, (tt + 1) * 512)
                hts = [f"hT{8 * half + 4 * tt + k}" for k in range(4)]
                for o2 in range(2):
                    dc = 2 * bb + o2
                    base = 4 * (step % 2)
                    ta, tb = rt[2 * (step % 2)], rt[2 * (step % 2) + 1]
                    na, nb_ = f"rt{2 * (step % 2)}", f"rt{2 * (step % 2) + 1}"
                    step += 1
                    csl = slice(o2 * 128, (o2 + 1) * 128)
                    for c in range(8):
                        S.pe(lambda e, c=c, base=base, csl=csl, g0=g0, s_gr=s_gr: e.matmul(P[:, base, :], lhsT=self.wsl[:, s_gr, c, csl], rhs=self.hT[:, c, g0:g0 + 512], start=(c == 0), stop=(c == 7)),
                             reads=[f"w{s_gr}"] + hts, writes=[f"ps{base}"])
                    for c in range(8):
                        S.pe(lambda e, c=c, base=base, csl=csl, g0=g0, s_gs=s_gs: e.matmul(P[:, base + 1, :], lhsT=self.wsl[:, s_gs, c, csl], rhs=self.hT[:, c, g0:g0 + 512], start=(c == 0), stop=(c == 7)),
                             reads=[f"w{s_gs}"] + hts, writes=[f"ps{base + 1}"])
                    for c in range(8):
                        S.pe(lambda e, c=c, base=base, csl=csl, lsl=lsl, s_br=s_br: e.matmul(P[:, base + 2, :], lhsT=self.wsl[:, s_br, c, csl], rhs=rT[:, c, lsl], start=(c == 0), stop=(c == 7)),
                             reads=[f"w{s_br}"] + [f"rT{4 * tt + k}" for k in range(4)], writes=[f"ps{base + 2}"])
                    for c in range(4):
                        S.pe(lambda e, c=c, base=base, csl=csl, g0=g0, s_bs=s_bs: e.matmul(P[:, base + 3, :], lhsT=self.wsl[:, s_bs, c, csl], rhs=uz[:, c, g0:g0 + 512], start=(c == 0), stop=(c == 3)),
                             reads=[f"w{s_bs}"] + [f"uz{c}_{(g0 // 512)}"], writes=[f"ps{base + 3}"])
                    S.act(lambda e, base=base, ta=ta: e.activation(out=ta, in_=P[:, base, :], func=AF.Sigmoid), writes=[f"ps{base}", na])
                    S.act(lambda e, base=base, tb=tb: e.activation(out=tb, in_=P[:, base + 1, :], func=AF.Sigmoid), writes=[f"ps{base + 1}", nb_])
                    S.dve(lambda e, base=base, ta=ta: e.tensor_tensor(out=ta, in0=P[:, base + 2, :], in1=ta, op=ALU.mult), reads=[na], writes=[f"ps{base + 2}", na])
                    S.dve(lambda e, base=base, tb=tb: e.tensor_tensor(out=tb, in0=P[:, base + 3, :], in1=tb, op=ALU.mult), reads=[nb_], writes=[f"ps{base + 3}", nb_])
                    S.dve(lambda e, ta=ta, tb=tb, dc=dc, lsl=lsl: e.tensor_tensor(out=mergedT[:, dc, lsl], in0=ta, in1=tb, op=ALU.add), reads=[na, nb_], writes=[f"mg{dc}_{tt}"])
        self.dump("mergedT", mergedT, [128, 8, 1024], [f"mg{dc}_{tt}" for dc in range(8) for tt in range(2)])
        if DEBUG["stop"] == "tailm":
            return
        for ob in range(2):
            self.load_wbig(dr["w_out"][l, :, 512 * ob:512 * (ob + 1)], 8, ob)
        xv = self.T8[:].rearrange("p a f -> p (a f)").rearrange("p (b d) -> p b d", b=2)
        for tl in range(8):
            T = 8 * half + tl
            pb = tl % 2
            xb = xv[:, pb, :]
            R_x = f"xb{pb}"
            S.dma("sp", lambda e, T=T, xb=xb: e.dma_start(out=xb, in_=self.spillA[T * 128:(T + 1) * 128, :]), writes=[R_x], key=f"rl{pb}")
            for ob in range(2):
                for c in range(8):
                    S.pe(lambda e, c=c, ob=ob, tl=tl: e.matmul(P[:, 6 + ob, :], lhsT=mergedT[:, c, tl * 128:(tl + 1) * 128], rhs=self.wbig(ob)[:, c, :], start=(c == 0), stop=(c == 7)),
                         reads=[f"w{2 * ob}", f"w{2 * ob + 1}"] + [f"mg{c}_{tl // 4}"], writes=[f"ps{6 + ob}"])
                S.dve(lambda e, ob=ob, xb=xb: e.scalar_tensor_tensor(out=xb[:, ob * 512:(ob + 1) * 512], in0=xb[:, ob * 512:(ob + 1) * 512], scalar=ALPHA, in1=P[:, 6 + ob, :], op0=ALU.mult, op1=ALU.add),
                      reads=[R_x], writes=[R_x, f"ps{6 + ob}"])
            self.ln_tile(xb, R_x, T, pb, router=True, make_hT=True)
            S.act(lambda e, xb=xb: e.activation(out=xb, in_=xb, func=AF.Identity, scale=ALPHA), reads=[R_x], writes=[R_x])
            S.dma("sp", lambda e, T=T, xb=xb: e.dma_start(out=self.spillB[T * 128:(T + 1) * 128, :], in_=xb), reads=[R_x], key=f"sb{pb}")

    def phase_moe(self, l, last):
        S, dr, P = self.S, self.dr, self.P
        h_tm = self.h_tm
        for T in range(NT):
            S.dma("sp", lambda e, T=T: e.dma_start(out=h_tm[:, T, :], in_=self.spillB[T * 128:(T + 1) * 128, :]), writes=[f"htm{T}"], key=f"ld{T % 4}")
        self.load_ln_params(1 + DEPTH + l)
        self.dump(f"hmix{l}_tm", h_tm, [128, NT, D], [f"htm{T}" for T in range(NT)])
        rtmp = self.uz[:].rearrange("p c t -> p (c t)").bitcast(F32)
        off = [0]
        def tmp(k, shape):
            n = int(np.prod(shape))
            ap = rtmp[:, off[0]:off[0] + n]
            off[0] += n
            if len(shape) == 2:
                return ap.rearrange("p (t a) -> p t a", t=NT)
            if len(shape) == 3:
                return ap.rearrange("p (t a b) -> p t a b", t=NT, a=shape[1])
            return ap
        Lg = self.logit
        G = Lg[:, :, 0:4]
        E4 = Lg[:, :, 4:36].rearrange("p t (g e) -> p t g e", g=4)
        lrs = [f"logit{T}" for T in range(NT)]
        gmax = tmp(0, [NT]); gsh = tmp(1, [NT, 4]); gsum = tmp(2, [NT]); oh = tmp(3, [NT, 4]); t4 = tmp(4, [NT, 4, 8])
        esel = tmp(5, [NT, 8]); m1 = tmp(6, [NT]); mk1 = tmp(7, [NT, 8]); e2 = tmp(8, [NT, 8]); m2 = tmp(9, [NT]); mk2 = tmp(10, [NT, 8])
        p2 = tmp(11, [NT]); w1 = tmp(12, [NT]); w2 = tmp(13, [NT]); wl = tmp(14, [NT, 8]); wl2 = tmp(15, [NT, 8])
        bc = lambda ap, n: ap.unsqueeze(2).broadcast_to([128, NT, n])
        S.dve(lambda e: e.tensor_reduce(out=gmax, in_=G, axis=AX.X, op=ALU.max), reads=lrs, writes=["r_gmax"])
        S.dve(lambda e: e.tensor_tensor(out=gsh, in0=G, in1=bc(gmax, 4), op=ALU.subtract), reads=lrs + ["r_gmax"], writes=["r_gsh"])
        S.dve(lambda e: e.tensor_tensor(out=oh, in0=G, in1=bc(gmax, 4), op=ALU.is_equal), reads=lrs + ["r_gmax"], writes=["r_oh"])
        S.act(lambda e: e.activation(out=gsh, in_=gsh, func=AF.Exp), reads=["r_gsh"], writes=["r_gsh"])
        S.dve(lambda e: e.tensor_reduce(out=gsum, in_=gsh, axis=AX.X, op=ALU.add), reads=["r_gsh"], writes=["r_gsum"])
        S.dve(lambda e: e.reciprocal(out=gsum, in_=gsum), reads=["r_gsum"], writes=["r_gsum"])
        S.dve(lambda e: e.tensor_tensor(out=t4, in0=E4, in1=oh.unsqueeze(3).broadcast_to([128, NT, 4, 8]), op=ALU.mult), reads=lrs + ["r_oh"], writes=["r_t4"])
        S.dve(lambda e: e.tensor_tensor(out=esel, in0=t4[:, :, 0, :], in1=t4[:, :, 1, :], op=ALU.add), reads=["r_t4"], writes=["r_esel"])
        S.dve(lambda e: e.tensor_tensor(out=esel, in0=esel, in1=t4[:, :, 2, :], op=ALU.add), reads=["r_t4", "r_esel"], writes=["r_esel"])
        S.dve(lambda e: e.tensor_tensor(out=esel, in0=esel, in1=t4[:, :, 3, :], op=ALU.add), reads=["r_t4", "r_esel"], writes=["r_esel"])
        S.dve(lambda e: e.tensor_reduce(out=m1, in_=esel, axis=AX.X, op=ALU.max), reads=["r_esel"], writes=["r_m1"])
        S.dve(lambda e: e.tensor_tensor(out=mk1, in0=esel, in1=bc(m1, 8), op=ALU.is_equal), reads=["r_esel", "r_m1"], writes=["r_mk1"])
        S.dve(lambda e: e.scalar_tensor_tensor(out=e2, in0=mk1, scalar=-1e30, in1=esel, op0=ALU.mult, op1=ALU.add), reads=["r_mk1", "r_esel"], writes=["r_e2"])
        S.dve(lambda e: e.tensor_reduce(out=m2, in_=e2, axis=AX.X, op=ALU.max), reads=["r_e2"], writes=["r_m2"])
        S.dve(lambda e: e.tensor_tensor(out=mk2, in0=e2, in1=bc(m2, 8), op=ALU.is_equal), reads=["r_e2", "r_m2"], writes=["r_mk2"])
        S.dve(lambda e: e.tensor_tensor(out=p2, in0=m2, in1=m1, op=ALU.subtract), reads=["r_m1", "r_m2"], writes=["r_p2"])
        S.act(lambda e: e.activation(out=p2, in_=p2, func=AF.Exp), reads=["r_p2"], writes=["r_p2"])
        S.dve(lambda e: e.tensor_scalar(out=w1, in0=p2, scalar1=1.0, scalar2=None, op0=ALU.add), reads=["r_p2"], writes=["r_w1"])
        S.dve(lambda e: e.reciprocal(out=w1, in_=w1), reads=["r_w1"], writes=["r_w1"])
        S.dve(lambda e: e.tensor_tensor(out=w2, in0=p2, in1=w1, op=ALU.mult), reads=["r_p2", "r_w1"], writes=["r_w2"])
        S.dve(lambda e: e.tensor_tensor(out=w1, in0=w1, in1=gsum, op=ALU.mult), reads=["r_w1", "r_gsum"], writes=["r_w1"])
        S.dve(lambda e: e.tensor_tensor(out=w2, in0=w2, in1=gsum, op=ALU.mult), reads=["r_w2", "r_gsum"], writes=["r_w2"])
        S.dve(lambda e: e.tensor_tensor(out=wl, in0=mk1, in1=bc(w1, 8), op=ALU.mult), reads=["r_mk1", "r_w1"], writes=["r_wl"])
        S.dve(lambda e: e.tensor_tensor(out=wl2, in0=mk2, in1=bc(w2, 8), op=ALU.mult), reads=["r_mk2", "r_w2"], writes=["r_wl2"])
        S.dve(lambda e: e.tensor_tensor(out=wl, in0=wl, in1=wl2, op=ALU.add), reads=["r_wl", "r_wl2"], writes=["r_wl"])
        comb4 = self.comb[:].rearrange("p t (g e) -> p t g e", g=4)
        S.dve(lambda e: e.tensor_tensor(out=comb4, in0=oh.unsqueeze(3).broadcast_to([128, NT, 4, 8]), in1=wl.unsqueeze(2).broadcast_to([128, NT, 4, 8]), op=ALU.mult),
              reads=["r_oh", "r_wl"], writes=["comb"])
        self.dump(f"comb{l}", self.comb[:], [128, NT, 32], ["comb"])
        wd = self.Y16[:, 0:6144].rearrange("p (s c n) -> p s c n", s=3, c=2)
        abuf = self.PH[:, 2048:4096].bitcast(BF16).rearrange("p (s k f n) -> p s k f n", s=2, k=2, f=2)
        steps = [(e_, tt) for e_ in range(NEXP) for tt in range(4)]
        info = {}

        def gateup(si):
            e_, tt = steps[si]
            if tt == 0:
                sg_ = self.load_w(dr["w_exp_gate"][l, e_], 8, 256)
                su_ = self.load_w(dr["w_exp_up"][l, e_], 8, 256)
                ws = e_ % 3
                S.dma("pool", lambda e, ws=ws, e_=e_: e.dma_start(out=wd[:, ws, :, :], in_=dr["w_exp_down"][l, e_].rearrange("(c p) n -> p c n", p=128)), writes=[f"wd{ws}"], key=f"wd{ws}")
                info[e_] = (sg_, su_, ws)
            sg_, su_, ws = info[e_]
            st_ = si % 2
            hts = [f"hT{4 * tt + k}" for k in range(4)]
            for fc in range(2):
                for kind, slot in ((0, sg_), (1, su_)):
                    bk = 2 * kind + fc
                    for c in range(8):
                        S.pe(lambda e, c=c, bk=bk, slot=slot, fc=fc, tt=tt: e.matmul(P[:, bk, :], lhsT=self.wsl[:, slot, c, fc * 128:(fc + 1) * 128], rhs=self.hT[:, c, tt * 512:(tt + 1) * 512], start=(c == 0), stop=(c == 7)),
                             reads=[f"w{slot}"] + hts, writes=[f"ps{bk}"])
            for fc in range(2):
                S.act(lambda e, fc=fc, st_=st_: e.activation(out=abuf[:, st_, 0, fc, :], in_=P[:, fc, :], func=AF.Silu), writes=[f"ps{fc}", f"ab{st_}sg{fc}"])
                S.dve(lambda e, fc=fc, st_=st_: e.tensor_tensor(out=abuf[:, st_, 1, fc, :], in0=P[:, 2 + fc, :], in1=abuf[:, st_, 0, fc, :], op=ALU.mult),
                      reads=[f"ab{st_}sg{fc}"], writes=[f"ps{2 + fc}", f"ab{st_}a{fc}"])

        dn = [0]

        def down(si):
            e_, tt = steps[si]
            sg_, su_, ws = info[e_]
            st_ = si % 2
            for ts in range(4):
                T = 4 * tt + ts
                for dh in range(2):
                    bk = 4 + dn[0] % 4
                    dn[0] += 1
                    for fc in range(2):
                        S.pe(lambda e, fc=fc, bk=bk, ts=ts, dh=dh, st_=st_, ws=ws: e.matmul(P[:, bk, :], lhsT=abuf[:, st_, 1, fc, ts * 128:(ts + 1) * 128], rhs=wd[:, ws, fc, dh * 512:(dh + 1) * 512], start=(fc == 0), stop=(fc == 1)),
                             reads=[f"ab{st_}a{fc}", f"wd{ws}"], writes=[f"ps{bk}"])
                    S.dve(lambda e, bk=bk, T=T, dh=dh, e_=e_: e.scalar_tensor_tensor(out=h_tm[:, T, dh * 512:(dh + 1) * 512], in0=P[:, bk, :], scalar=self.comb[:, T, e_:e_ + 1], in1=h_tm[:, T, dh * 512:(dh + 1) * 512], op0=ALU.mult, op1=ALU.add),
                          reads=["comb", f"htm{T}"], writes=[f"htm{T}", f"ps{bk}"])

        nexp_run = int(os.environ.get("NEXP_RUN", NEXP))
        nsteps = nexp_run * 4
        for si in range(nsteps + 1):
            if si < nsteps:
                gateup(si)
            if si >= 1:
                down(si - 1)
        for T in range(NT):
            self.ln_tile(h_tm[:, T, :], f"htm{T}", T, T % 2, router=False, make_hT=not last)
            if last:
                S.dma("sp", lambda e, T=T: e.dma_start(out=self.out[T * 128:(T + 1) * 128, :], in_=h_tm[:, T, :]), reads=[f"htm{T}"], key=f"out{T % 4}")
            else:
                S.dma("sp", lambda e, T=T: e.dma_start(out=self.spillA[T * 128:(T + 1) * 128, :], in_=h_tm[:, T, :]), reads=[f"htm{T}"], key=f"sp{T % 4}")
        self.dump(f"hffn{l}_tm", h_tm, [128, NT, D], [f"htm{T}" for T in range(NT)])


def build(nc, st):
    B = Builder(nc, st)
    B.consts()
    B.phase_in()
    B.S.barrier()
    if DEBUG["stop"] == "in":
        return B
    for l in range(DEPTH):
        B.phase_s5(l)
        B.S.barrier()
        if DEBUG["stop"] == "s5":
            return B
        for half in range(2):
            B.phase_ret(l, half)
            B.S.barrier()
            if DEBUG["stop"] in ("ret", "retqk", "retv", "retr"):
                return B
            B.phase_tail(l, half)
            B.S.barrier()
            if DEBUG["stop"] in ("tail", "tailm"):
                return B
        B.phase_moe(l, last=(l == DEPTH - 1))
        B.S.barrier()
        if DEBUG["stop"] == f"moe{l}":
            return B
    return B


def kernel(**inputs):
    x = np.ascontiguousarray(np.asarray(inputs["x"], dtype=np.float32))
    params = host_layout(inputs)
    params.update(host_constants())
    nc = bass.Bass("TRN2", target_bir_lowering=False)
    with ExitStack() as st:
        B = build(nc, st)
        B.S.emit(st)
    in_maps = []
    for b in range(8):
        m = {"x": x[b]}
        m.update(params)
        in_maps.append(m)
    res = run_bass_kernel_spmd(nc, in_maps, core_ids=list(range(8)))
    return np.stack([np.asarray(r["out"], dtype=np.float32) for r in res.results], axis=0)
```
